# Optimizing a Trainium2 kernel written in Bass

```python
import jax, jax.numpy as jnp
from jax import lax
import numpy as np

D_MODEL = 1024
BATCH = 2
SEQ = 8192
DEPTH = 1

D_MIX = D_MODEL
N_HEADS = 8
N_KV_HEADS = 2
HEAD_DIM = 64
D_ATTN = N_HEADS * HEAD_DIM
D_KV = N_KV_HEADS * HEAD_DIM
WINDOW = 128
BLOCK = 128
D_GMLP = D_MIX - D_ATTN
CHUNK = 128
N_GMLP_GROUPS = 8
GMLP_GROUP_DIM = D_GMLP // N_GMLP_GROUPS
D_IN = D_ATTN + 2 * D_KV + 2 * D_GMLP
N_EXPERTS = 16
CAPACITY_FACTOR = 2
D_FF_EXPERT = 2816
EPS = 1e-6
MASK_VALUE = -1e30

kernel_name = "hymba_ec_hybrid_encoder_block"


def rms_norm(x, g):
    xf = x.astype(jnp.float32)
    y = xf * lax.rsqrt(jnp.mean(xf * xf, axis=-1, keepdims=True) + EPS)
    return (y * g.astype(jnp.float32)).astype(x.dtype)


def layer_norm(x, g, b):
    xf = x.astype(jnp.float32)
    mu = jnp.mean(xf, axis=-1, keepdims=True)
    xc = xf - mu
    y = xc * lax.rsqrt(jnp.mean(xc * xc, axis=-1, keepdims=True) + EPS)
    return (y * g.astype(jnp.float32) + b.astype(jnp.float32)).astype(x.dtype)


def modulate(h, shift, scale):
    return h * (1 + scale[:, None, :]) + shift[:, None, :]


def alibi_slopes(n):
    return jnp.exp2(-8.0 * jnp.arange(1, n + 1, dtype=jnp.float32) / n)


def windowed_gqa(q, k, v, sink):
    b, s = q.shape[:2]
    nb = s // BLOCK
    g = N_HEADS // N_KV_HEADS
    qb = q.reshape(b, nb, BLOCK, N_KV_HEADS, g, HEAD_DIM)

    def band(t):
        tp = jnp.pad(t, ((0, 0), (BLOCK, BLOCK), (0, 0), (0, 0)))
        tp = tp.reshape(b, nb + 2, BLOCK, N_KV_HEADS, HEAD_DIM)
        return jnp.concatenate([tp[:, :-2], tp[:, 1:-1], tp[:, 2:]], axis=2)

    kb, vb = band(k), band(v)
    scores = jnp.einsum('bnqkgd,bnskd->bnkgqs', qb, kb).astype(jnp.float32) * (HEAD_DIM ** -0.5)
    blk = jnp.arange(nb)[:, None] * BLOCK
    q_pos = blk + jnp.arange(BLOCK)[None, :]
    k_pos = blk - BLOCK + jnp.arange(3 * BLOCK)[None, :]
    dist = jnp.abs(q_pos[:, :, None] - k_pos[:, None, :])
    valid = (dist <= WINDOW) & (k_pos[:, None, :] >= 0) & (k_pos[:, None, :] < s)
    slopes = alibi_slopes(N_HEADS).reshape(N_KV_HEADS, g)
    scores = scores - slopes[:, :, None, None] * dist[None, :, None, None].astype(jnp.float32)
    scores = jnp.where(valid[None, :, None, None], scores, MASK_VALUE)
    sink_l = sink.astype(jnp.float32).reshape(N_KV_HEADS, g)[:, :, None]
    m = jnp.maximum(scores.max(axis=-1), sink_l)
    p = jnp.exp(scores - m[..., None])
    denom = p.sum(axis=-1) + jnp.exp(sink_l - m)
    probs = (p / denom[..., None]).astype(v.dtype)
    out = jnp.einsum('bnkgqs,bnskd->bnqkgd', probs, vb)
    return out.reshape(b, s, D_ATTN)


def spatial_gating(u, v, ln_g, ln_b, w_s, b_s):
    b, s = v.shape[:2]
    nb = s // CHUNK
    vn = layer_norm(v, ln_g, ln_b).reshape(b, nb, CHUNK, N_GMLP_GROUPS, GMLP_GROUP_DIM)
    z = jnp.einsum('gts,bnsgc->bntgc', w_s, vn) + b_s.T[:, :, None]
    return u * z.reshape(b, s, D_GMLP)


def expert_choice_ffn(h, w_router, w_gate, w_up, w_down):
    b, s, d = h.shape
    cap = CAPACITY_FACTOR * s // N_EXPERTS
    aff = jax.nn.softmax((h @ w_router).astype(jnp.float32), axis=-1)
    gates, idx = lax.top_k(jnp.swapaxes(aff, 1, 2), cap)
    xs = jax.vmap(lambda hb, ib: hb[ib])(h, idx)
    hid = jax.nn.silu(jnp.einsum('becd,edf->becf', xs, w_gate)) * jnp.einsum('becd,edf->becf', xs, w_up)
    out = jnp.einsum('becf,efd->becd', hid, w_down) * gates[..., None].astype(h.dtype)
    return jax.vmap(lambda ob, ib: jnp.zeros((s, d), h.dtype).at[ib.reshape(-1)].add(ob.reshape(-1, d)))(out, idx)


def setup_inputs(seed: int = 0) -> dict:
    key = jax.random.key(seed)
    ks = jax.random.split(key, 24)
    nrm = jax.random.normal
    L, D = DEPTH, D_MODEL
    return {
        "x": nrm(ks[0], (BATCH, SEQ, D), jnp.float32),
        "c": nrm(ks[1], (BATCH, D), jnp.float32),
        "w_ada": nrm(ks[2], (L, D, 6 * D), jnp.float32) * (0.5 * D ** -0.5),
        "b_ada": 0.01 * nrm(ks[3], (L, 6 * D), jnp.float32),
        "norm_pre_mix": 1.0 + 0.1 * nrm(ks[4], (L, D), jnp.float32),
        "norm_post_mix": 1.0 + 0.1 * nrm(ks[5], (L, D), jnp.float32),
        "w_in": nrm(ks[6], (L, D, D_IN), jnp.float32) * D ** -0.5,
        "sink": 0.5 * nrm(ks[7], (L, N_HEADS), jnp.float32),
        "sgu_ln_g": 1.0 + 0.1 * nrm(ks[8], (L, D_GMLP), jnp.float32),
        "sgu_ln_b": 0.01 * nrm(ks[9], (L, D_GMLP), jnp.float32),
        "w_s": nrm(ks[10], (L, N_GMLP_GROUPS, CHUNK, CHUNK), jnp.float32) * CHUNK ** -0.5,
        "b_s": 1.0 + 0.1 * nrm(ks[11], (L, N_GMLP_GROUPS, CHUNK), jnp.float32),
        "norm_out_attn": 1.0 + 0.1 * nrm(ks[12], (L, D_ATTN), jnp.float32),
        "norm_out_gmlp": 1.0 + 0.1 * nrm(ks[13], (L, D_GMLP), jnp.float32),
        "w_out": nrm(ks[14], (L, D_MIX, D), jnp.float32) * D_MIX ** -0.5,
        "norm_pre_ffn": 1.0 + 0.1 * nrm(ks[15], (L, D), jnp.float32),
        "norm_post_ffn": 1.0 + 0.1 * nrm(ks[16], (L, D), jnp.float32),
        "w_router": nrm(ks[17], (L, D, N_EXPERTS), jnp.float32) * D ** -0.5,
        "w_gate": nrm(ks[18], (L, N_EXPERTS, D, D_FF_EXPERT), jnp.float32) * D ** -0.5,
        "w_up": nrm(ks[19], (L, N_EXPERTS, D, D_FF_EXPERT), jnp.float32) * D ** -0.5,
        "w_down": nrm(ks[20], (L, N_EXPERTS, D_FF_EXPERT, D), jnp.float32) * D_FF_EXPERT ** -0.5,
    }


def reference(x, c, w_ada, b_ada, norm_pre_mix, norm_post_mix, w_in, sink, sgu_ln_g, sgu_ln_b,
              w_s, b_s, norm_out_attn, norm_out_gmlp, w_out, norm_pre_ffn, norm_post_ffn,
              w_router, w_gate, w_up, w_down):
    b, s, _ = x.shape
    for l in range(DEPTH):
        mod = jax.nn.silu(c) @ w_ada[l] + b_ada[l]
        shift1, scale1, gate1, shift2, scale2, gate2 = jnp.split(mod, 6, axis=-1)

        h = modulate(rms_norm(x, norm_pre_mix[l]), shift1, scale1)
        proj = h @ w_in[l]
        q, k, v, zg = jnp.split(proj, [D_ATTN, D_ATTN + D_KV, D_ATTN + 2 * D_KV], axis=-1)
        attn = windowed_gqa(q.reshape(b, s, N_HEADS, HEAD_DIM),
                            k.reshape(b, s, N_KV_HEADS, HEAD_DIM),
                            v.reshape(b, s, N_KV_HEADS, HEAD_DIM), sink[l])
        u, vg = jnp.split(jax.nn.gelu(zg), 2, axis=-1)
        gm = spatial_gating(u, vg, sgu_ln_g[l], sgu_ln_b[l], w_s[l], b_s[l])
        mix = jnp.concatenate([rms_norm(attn, norm_out_attn[l]), rms_norm(gm, norm_out_gmlp[l])], axis=-1)
        x = x + gate1[:, None, :] * rms_norm(mix @ w_out[l], norm_post_mix[l])

        h2 = modulate(rms_norm(x, norm_pre_ffn[l]), shift2, scale2)
        y = expert_choice_ffn(h2, w_router[l], w_gate[l], w_up[l], w_down[l])
        x = x + gate2[:, None, :] * rms_norm(y, norm_post_ffn[l])
    return x
```

```python
import numpy as np
import ml_dtypes
import concourse.bass as bass
import concourse.mybir as mybir
from concourse.bass_utils import run_bass_kernel_spmd

F32 = mybir.dt.float32
BF16 = mybir.dt.bfloat16
I32 = mybir.dt.int32
AF = mybir.ActivationFunctionType
ALU = mybir.AluOpType
AX = mybir.AxisListType

D = 1024
SEQ = 8192
NT = 16
NE = 16
FF = 2816
NFC = FF // 128
CAP = 1024
EPS = 1e-6
STAGE = 4


class Sched:
    ENGS = ("pe", "act", "dve", "pool", "sp")

    def __init__(self, nc, tag):
        self.nc = nc
        self.tag = tag
        self.q = {e: [] for e in self.ENGS}
        self.cnt = {e: 0 for e in self.ENGS}
        self.last_w = {}
        self.readers = {}
        self.chan = {}
        self.sems = {}

    def _deps(self, r, w):
        deps = set()
        for k in r:
            if k in self.last_w:
                deps.add(self.last_w[k])
        for k in w:
            if k in self.last_w:
                deps.add(self.last_w[k])
            deps |= self.readers.get(k, set())
        return deps

    def _commit(self, r, w, tk):
        for k in r:
            self.readers.setdefault(k, set()).add(tk)
        for k in w:
            self.last_w[k] = tk
            self.readers[k] = set()

    def op(self, eng, fn, r=(), w=()):
        r = list(r); w = list(w)
        w += [k for k in r if k.startswith("bk") and k not in w]
        deps = self._deps(r, w)
        self.cnt[eng] += 1
        tk = ("e_" + eng, self.cnt[eng])
        if eng == "pe":
            deps = {d for d in deps if d[0] != "e_pe"}
        self.q[eng].append((fn, deps, tk[0], 1))
        self._commit(r, w, tk)
        return tk

    def dma(self, eng, ch, fn, r=(), w=(), group=False):
        deps = self._deps(r, w)
        self.chan[ch] = self.chan.get(ch, 0) + 1
        tk = ("c_" + ch, -1 if group else self.chan[ch] * 16)
        self.q[eng].append((fn, deps, tk[0], 16))
        self._commit(r, w, tk)
        return tk

    def coll(self, ch, fn, r=(), w=()):
        deps = self._deps(r, w)
        self.chan[ch] = self.chan.get(ch, 0) + 1
        tk = ("c_" + ch, self.chan[ch])
        self.q["pool"].append((fn, deps, tk[0], 1))
        self._commit(r, w, tk)
        return tk

    def sem_names(self):
        names = ["e_" + e for e in self.ENGS if self.cnt[e] > 0]
        names += ["c_" + c for c in self.chan]
        return names

    def emit(self, sems, final_waits=()):
        nc = self.nc
        with nc.Block() as block:
            def mk(engname):
                def body(e):
                    waited = {}
                    for (fn, deps, semname, inc) in self.q[engname]:
                        deps = {(sn, (self.chan[sn[2:]] * 16 if val == -1 else val)) for (sn, val) in deps}
                        for (sn, val) in sorted(deps):
                            if waited.get(sn, 0) >= val:
                                continue
                            e.wait_ge(sems[sn], val)
                            waited[sn] = val
                        ins = fn(e)
                        if inc == 1 and semname.startswith("c_"):
                            ins.then_inc(sems[semname])
                        else:
                            ins.then_inc(sems[semname], inc)
                    if engname == "sp":
                        for (sn, val) in final_waits:
                            e.wait_ge(sems[sn], val)
                return body
            if self.q["pe"]:
                block.tensor(mk("pe"))
            if self.q["act"]:
                block.scalar(mk("act"))
            if self.q["dve"]:
                block.vector(mk("dve"))
            if self.q["pool"]:
                block.gpsimd(mk("pool"))
            if self.q["sp"] or final_waits:
                block.sync(mk("sp"))


def bcast(ap, shape):
    return ap.to_broadcast(shape)


PROWA = dict(g_pre=0, g_post=1024, g_pre2=2048, g_post2=3072)
PROW = dict(ln_g=0, ln_b=512, g_attn=1024, g_gmlp=1536, sink=2048, edge=2056, eps=2058)
NPROW = 2064


def build_program(stage=STAGE, cut=99):
    nc = bass.Bass("TRN2", target_bir_lowering=False)
    dt = nc.dram_tensor
    xin = dt("xin", [NT + 2, 128, D], F32, kind="ExternalInput").ap()
    cvec = dt("cvec", [128, 8], F32, kind="ExternalInput").ap()
    w_ada = dt("w_ada", [8, 128, 6 * D], F32, kind="ExternalInput").ap()
    b_ada = dt("b_ada", [1, 6 * D], F32, kind="ExternalInput").ap()
    w_in = dt("w_in", [8, 128, 1792], F32, kind="ExternalInput").ap()
    w_out = dt("w_out", [8, 128, D], F32, kind="ExternalInput").ap()
    prow = dt("prow", [128, NPROW], F32, kind="ExternalInput").ap()
    prowA = dt("prowA", [128, 4 * D], F32, kind="ExternalInput").ap()
    biast = dt("biast", [128, 8, 384], F32, kind="ExternalInput").ap()
    w_sT = dt("w_sT", [128, 8, 128], F32, kind="ExternalInput").ap()
    b_sT = dt("b_sT", [128, 8], F32, kind="ExternalInput").ap()
    w_r = dt("w_r", [8, 128, NE], F32, kind="ExternalInput").ap()
    ident = dt("ident", [128, 128], F32, kind="ExternalInput").ap()
    if stage == 3:
      wg = dt("wg", [NE, NFC, 128, 8, 128], F32, kind="ExternalInput").ap()
      wu = dt("wu", [NE, NFC, 128, 8, 128], F32, kind="ExternalInput").ap()
      wd = dt("wd", [NE, NFC, 128, D], F32, kind="ExternalInput").ap()
      selm = dt("selm", [128, 128], F32, kind="ExternalInput").ap()
      selown = dt("selown", [128, NE], F32, kind="ExternalInput").ap()
    if stage >= 4:
      wg = dt("wg", [2, NFC, 128, 8, 128], F32, kind="ExternalInput").ap()
      wu = dt("wu", [2, NFC, 128, 8, 128], F32, kind="ExternalInput").ap()
      wd = dt("wd", [2, NFC, 128, D], F32, kind="ExternalInput").ap()
      selm = dt("selm", [128, 128], F32, kind="ExternalInput").ap()
      selown = dt("selown", [128, NE], F32, kind="ExternalInput").ap()
      selq = dt("selq", [128, 128], F32, kind="ExternalInput").ap()
      selpair = dt("selpair", [128, NE], F32, kind="ExternalInput").ap()
      iota_d = dt("iota_d", [128, CAP], F32, kind="ExternalInput").ap()
      tokid_d = dt("tokid_d", [128, 64, 2], F32, kind="ExternalInput").ap()
      w2_d = dt("w2_d", [2, 1], F32, kind="ExternalInput").ap()
      cbase_d = dt("cbase_d", [128, NE], F32, kind="ExternalInput").ap()
      pbase_d = dt("pbase_d", [128, 32], F32, kind="ExternalInput").ap()
      h2loc = dt("h2loc", [NT * 128, D], BF16)
      h2all = dt("h2all", [8 * NT * 128, D], BF16)
      outloc = dt("outloc", [4 * CAP, D], BF16)
      outall = dt("outall", [8 * 4 * CAP, D], BF16)
    out = dt("out", [NT, 128, D], F32, kind="ExternalOutput").ap()
    x1d = dt("x1d", [NT, 128, D], F32)
    h2Td = dt("h2Td", [128, 8, NT * 128], F32)
    agi = dt("agi", [NE, NT * 128], F32)
    ago = dt("ago", [8 * NE, NT * 128], F32)

    from contextlib import ExitStack
    with ExitStack() as gs:
        def SB(name, shape, dtype=F32, stack=gs):
            return stack.enter_context(nc.sbuf_tensor(name, shape, dtype))

        def PS(name, shape, dtype=F32, stack=gs):
            return stack.enter_context(nc.psum_tensor(name, shape, dtype))

        mod = SB("mod", [128, 6 * D])
        pr = SB("pr", [128, NPROW])
        idf = SB("idf", [128, 128])
        idb = SB("idb", [128, 128], BF16)
        affo = SB("affo", [128, NT, NE])
        gm_all = SB("gm_all", [128, NT, NE])
        lidx = SB("lidx", [128, 32], I32)
        cidx = SB("cidx", [128, NT * NE], I32)
        banks = [PS(f"bank{i}", [128, 512]) for i in range(8)]

        def bank_bf(i):
            return banks[i][:].bitcast(BF16)

        SH1, GS1, GG1, SH2, GS2, GG2 = [slice(i * D, (i + 1) * D) for i in range(6)]

        with ExitStack() as s0:
            S = Sched(nc, "b0")
            cv = SB("cv", [128, 8], F32, s0)
            scb = SB("scb", [128, 8, 128], F32, s0)
            bada = SB("bada", [1, 2, 512], F32, s0)
            ones1 = SB("ones1", [1, 128], F32, s0)
            wab = SB("wab", [128, 2, 8, 512], F32, s0)
            prA = SB("prA", [128, 4 * D], F32, s0)
            S.dma("sp", "ld0", group=True, fn=lambda e: e.dma_start(out=cv[:], in_=cvec[:, :]), w=["cv"])
            S.dma("sp", "ld0", group=True, fn=lambda e: e.dma_start(out=prA[:], in_=prowA[:, :]), w=["prA"])
            S.dma("sp", "ld0", group=True, fn=lambda e: e.dma_start(out=pr[:], in_=prow[:, :]), w=["pr"])
            S.dma("sp", "ld0", group=True, fn=lambda e: e.dma_start(out=idf[:], in_=ident[:, :]), w=["idf"])
            S.op("dve", lambda e: e.tensor_copy(out=idb[:], in_=idf[:]), r=["idf"], w=["idb"])
            S.op("pool", lambda e: e.memset(ones1[:], 1.0), w=["ones1"])
            S.op("act", lambda e: e.activation(out=cv[:], in_=cv[:], func=AF.Silu), r=["cv"], w=["cv"])
            S.op("dve", lambda e: e.tensor_copy(out=scb[:], in_=bcast(cv[:, :].unsqueeze(2), [128, 8, 128])),
                 r=["cv"], w=["scb"])
            for nb in range(12):
                pa = nb % 2
                S.dma("sp", f"wa{pa}",
                      lambda e, nb=nb, pa=pa: e.dma_start(
                          out=wab[:, pa, :, :],
                          in_=w_ada[:, :, nb * 512:(nb + 1) * 512].rearrange("k p n -> p k n")),
                      w=[f"wab{pa}"])
                S.dma("sp", f"ba{pa}",
                      lambda e, nb=nb, pa=pa: e.dma_start(out=bada[0:1, pa, :], in_=b_ada[0:1, nb * 512:(nb + 1) * 512]),
                      w=[f"bada{pa}"])
                bk = banks[nb % 2]
                for kc in range(8):
                    S.op("pe", lambda e, kc=kc, pa=pa, bk=bk: e.matmul(
                        bk[:, :], lhsT=scb[:, kc, :], rhs=wab[:, pa, kc, :], start=(kc == 0), stop=False),
                        r=[f"wab{pa}", "scb"], w=[f"bk{nb % 2}"])
                S.op("pe", lambda e, pa=pa, bk=bk: e.matmul(
                    bk[:, :], lhsT=ones1[0:1, :], rhs=bada[0:1, pa, :], start=False, stop=True),
                    r=["ones1", f"bada{pa}"], w=[f"bk{nb % 2}"])
                S.op("act", lambda e, nb=nb, bk=bk: e.copy(out=mod[:, nb * 512:(nb + 1) * 512], in_=bk[:, :]),
                     r=[f"bk{nb % 2}"], w=[f"mod{nb // 2}"])

            def fold(mi, sl, gname, plus1):
                g = prA[:, PROWA[gname]:PROWA[gname] + D]
                if plus1:
                    S.op("dve", lambda e: e.scalar_tensor_tensor(out=mod[:, sl], in0=mod[:, sl], scalar=1.0, in1=g,
                                                                 op0=ALU.add, op1=ALU.mult),
                         r=[f"mod{mi}", "prA"], w=[f"mod{mi}"])
                else:
                    S.op("dve", lambda e: e.tensor_tensor(out=mod[:, sl], in0=mod[:, sl], in1=g, op=ALU.mult),
                         r=[f"mod{mi}", "prA"], w=[f"mod{mi}"])
            fold(1, GS1, "g_pre", True)
            fold(2, GG1, "g_post", False)
            fold(4, GS2, "g_pre2", True)
            fold(5, GG2, "g_post2", False)
            fw0 = []
            if stage == 0:
                for i in range(6):
                    S.dma("sp", "st0", lambda e, i=i: e.dma_start(out=out[i, :, :], in_=mod[:, i * D:(i + 1) * D]),
                          r=[f"mod{i}"], w=["outd"])
                fw0 = [("c_st0", 96)]
            sems = {n: gs.enter_context(nc.semaphore(f"b0_{n}")) for n in S.sem_names()}
            S.emit(sems, final_waits=fw0)
        if stage == 0:
            return nc

        with ExitStack() as s1:
            S = Sched(nc, "b1")
            win = SB("win", [128, 8, 1792], BF16, s1)
            wout = SB("wout", [128, 8, D], BF16, s1)
            wsb = SB("wsb", [128, 8, 128], BF16, s1)
            bsb = SB("bsb", [128, 8], F32, s1)
            wrs = SB("wrs", [128, 8, NE], F32, s1)
            bt = SB("bt", [128, 8, 384], F32, s1)
            xb = SB("xb", [128, 3, D], F32, s1)
            st = SB("st", [128, 2, 32], F32, s1)
            stA = SB("stA", [128, 3, 8], F32, s1)
            hbf = SB("hbf", [128, 1, D], BF16, s1)
            hT = SB("hT", [128, 2, 8, 128], BF16, s1)
            qT = SB("qT", [128, 3, 4, 128], BF16, s1)
            kT = SB("kT", [128, NT + 2, 128], BF16, s1)
            vv = SB("vv", [128, NT + 2, 128], BF16, s1)
            ug = SB("ug", [128, 3, 512], BF16, s1)
            vg = SB("vg", [128, 3, 512], F32, s1)
            sall = SB("sall", [128, 8, 384], F32, s1)
            pbf = SB("pbf", [128, 8, 384], BF16, s1)
            pT = SB("pT", [128, 8, 384], BF16, s1)
            attn = SB("attn", [128, 512], F32, s1)
            junkA = SB("junkA", [128, D], BF16, s1)
            junkB = SB("junkB", [128, D], BF16, s1)
            vn = SB("vn", [128, 512], BF16, s1)
            gmt = SB("gmt", [128, 512], F32, s1)
            mix = SB("mix", [128, D], BF16, s1)
            mixT = SB("mixT", [128, 8, 128], BF16, s1)
            tmpA = SB("tmpA", [128, D], F32, s1)
            tmpB = SB("tmpB", [128, D], F32, s1)
            h2f = SB("h2f", [128, D], F32, s1)
            h2Tf = SB("h2Tf", [128, 8, 128], F32, s1)
            affT = SB("affT", [NE, NT * 128], F32, s1)
            sm = SB("sm", [128, 8], F32, s1)
            import os
            LM = int(os.environ.get("LM", "31"))
            S.dma("sp", "ld0", group=True, fn=lambda e: e.dma_start(out=bt[:], in_=biast[:, :, :]), w=["bt"])
            S.dma("sp", "ld0", group=True, fn=lambda e: e.dma_start(out=bsb[:], in_=b_sT[:, :]), w=["bsb"])
            if LM & 2:
                S.dma("sp", "ld0", group=True, fn=lambda e: e.dma_start(out=wrs[:], in_=w_r.rearrange("k p n -> p k n")), w=["wrs"])
            wstg = SB("wstg", [128, 2, 1024], F32, s1)
            S.dma("sp", "ws1", lambda e: e.dma_start(out=wstg[:, 1, 0:1024], in_=w_sT.rearrange("p g t -> p (g t)")),
                  w=["wstg1"])
            S.op("act", lambda e: e.copy(out=wsb[:, :, :].rearrange("p g t -> p (g t)"), in_=wstg[:, 1, 0:1024]),
                 r=["wstg1"], w=["wsb"])
            for kc in range(8):
                for hh in range(2):
                    sp_ = hh
                    c0 = hh * 896
                    S.dma("sp", f"ws{sp_}", lambda e, kc=kc, sp_=sp_, c0=c0: e.dma_start(
                        out=wstg[:, sp_, 0:896], in_=w_in[kc, :, c0:c0 + 896]), w=[f"wstg{sp_}"])
                    if hh:
                        S.op("act", lambda e, kc=kc, sp_=sp_, c0=c0: e.copy(out=win[:, kc, c0:c0 + 896], in_=wstg[:, sp_, 0:896]),
                             r=[f"wstg{sp_}"], w=["win"])
                    else:
                        S.op("dve", lambda e, kc=kc, sp_=sp_, c0=c0: e.tensor_copy(out=win[:, kc, c0:c0 + 896], in_=wstg[:, sp_, 0:896]),
                             r=[f"wstg{sp_}"], w=["win"])
            for kc in range(8):
                sp_ = kc % 2
                S.dma("sp", f"ws{sp_}", lambda e, kc=kc, sp_=sp_: e.dma_start(out=wstg[:, sp_, 0:D], in_=w_out[kc, :, :]),
                      w=[f"wstg{sp_}"])
                S.op("act" if kc % 2 else "dve", (lambda e, kc=kc, sp_=sp_: e.copy(out=wout[:, kc, :], in_=wstg[:, sp_, 0:D])) if kc % 2 else
                     (lambda e, kc=kc, sp_=sp_: e.tensor_copy(out=wout[:, kc, :], in_=wstg[:, sp_, 0:D])),
                     r=[f"wstg{sp_}"], w=["wout"])
            epsc = pr[:, PROW["eps"]:PROW["eps"] + 1]

            def rstd_ops(S, ssq_ap, out_ap, n, keys_r, keys_w):
                S.op("act", lambda e: e.activation(out=out_ap, in_=ssq_ap, func=AF.Ln, scale=1.0 / n, bias=epsc),
                     r=keys_r + ["pr"], w=keys_w)
                S.op("act", lambda e: e.activation(out=out_ap, in_=out_ap, func=AF.Exp, scale=-0.5),
                     r=keys_w, w=keys_w)

            def dump(S, slot, ap, keys, n):
                S.dma("sp", "st1", lambda e: e.dma_start(out=out[slot, :, 0:n], in_=ap), r=keys, w=["outd"])

            def stageA(j, S):
                pa = j % 2
                xs = j % 3
                p3 = j % 3
                halo = (j == 0 or j == NT + 1)
                x_t = xb[:, xs, :]
                if cut == -1:
                    if j == 0:
                        dump(S, 0, bt[:, 0, :], ["bt"], 384)
                        dump(S, 1, mod[:, 0:384], [], 384)
                        dump(S, 2, pr[:, 0:384], [], 384)
                        dump(S, 3, bt[:, 1, :], ["bt"], 384)
                    return
                S.dma("sp", f"x{xs}", lambda e: e.dma_start(out=x_t, in_=xin[j, :, :]), w=[f"x{xs}"])
                ssq = stA[:, p3, 0:1]
                rs = stA[:, p3, 1:2]
                S.op("pool", lambda e: e.memset(ssq, 0.0), w=[f"ssqA{p3}"])
                S.op("act", lambda e: e.activation(out=junkA[:], in_=x_t, func=AF.Square, accum_out=ssq),
                     r=[f"x{xs}"], w=["junkA", f"ssqA{p3}"])
                rstd_ops(S, ssq, rs, D, [f"ssqA{p3}"], [f"rsA{p3}"])
                S.op("dve", lambda e: e.scalar_tensor_tensor(out=tmpA[:], in0=x_t, scalar=rs, in1=mod[:, GS1],
                                                             op0=ALU.mult, op1=ALU.mult),
                     r=[f"x{xs}", f"rsA{p3}", "mod1"], w=["tmpA"])
                S.op("dve", lambda e: e.tensor_tensor(out=hbf[:, 0, :], in0=tmpA[:], in1=mod[:, SH1], op=ALU.add),
                     r=["tmpA", "mod0"], w=["hbf"])
                if cut == -2:
                    if j == 0:
                        dump(S, 0, tmpA[:, :], ["hbf", "tmpA"], 1024)
                    return
                tb = bank_bf(0)
                for kc in range(8):
                    S.op("pe", lambda e, kc=kc: e.transpose(out=tb[:, kc * 128:(kc + 1) * 128],
                                                            in_=hbf[:, 0, kc * 128:(kc + 1) * 128], identity=idb[:]),
                         r=["hbf", "idb"], w=["bk0"])
                S.op("act", lambda e: e.copy(out=hT[:, pa, :, :], in_=tb[:, 0:1024].rearrange("p (k t) -> p k t", k=8)),
                     r=["bk0"], w=[f"hT{pa}"])
                if cut == -3:
                    if j == 0:
                        dump(S, 0, tmpA[:, :], [f"hT{pa}", "tmpA"], 1024)
                    return
                for kc in range(8):
                    S.op("pe", lambda e, kc=kc: e.matmul(banks[1][:, 0:128], lhsT=hT[:, pa, kc, :],
                                                         rhs=win[:, kc, 640:768], start=(kc == 0), stop=(kc == 7)),
                         r=[f"hT{pa}", "win"], w=["bk1"])
                for kc in range(8):
                    S.op("pe", lambda e, kc=kc: e.matmul(banks[1][:, 128:256], lhsT=win[:, kc, 512:640],
                                                         rhs=hT[:, pa, kc, :], start=(kc == 0), stop=(kc == 7)),
                         r=[f"hT{pa}", "win"], w=["bk1"])
                S.op("dve", lambda e: e.tensor_copy(out=vv[:, j, :], in_=banks[1][:, 0:128]), r=["bk1"], w=[f"v{j}"])
                S.op("act", lambda e: e.copy(out=kT[:, j, :], in_=banks[1][:, 128:256]), r=["bk1"], w=[f"k{j}"])
                if cut == -4:
                    if j == 0:
                        dump(S, 0, tmpA[:, :], [f"v{j}", f"k{j}", "tmpA"], 1024)
                    return
                if halo:
                    return
                for m in range(4):
                    for kc in range(8):
                        S.op("pe", lambda e, kc=kc, m=m: e.matmul(
                            banks[2][:, m * 128:(m + 1) * 128], lhsT=win[:, kc, m * 128:(m + 1) * 128],
                            rhs=hT[:, pa, kc, :], start=(kc == 0), stop=(kc == 7)),
                            r=[f"hT{pa}", "win"], w=["bk2"])
                S.op("act", lambda e: e.copy(out=qT[:, p3, :, :],
                                             in_=banks[2][:, :].rearrange("p (m t) -> p m t", m=4)),
                     r=["bk2"], w=[f"qT{p3}"])
                for (bi, c0, dst, key) in ((3, 768, ug, "ug"), (3, 1280, vg, "vg")):
                    for kc in range(8):
                        S.op("pe", lambda e, kc=kc, bi=bi, c0=c0: e.matmul(
                            banks[bi][:, :], lhsT=hT[:, pa, kc, :], rhs=win[:, kc, c0:c0 + 512],
                            start=(kc == 0), stop=(kc == 7)),
                            r=[f"hT{pa}", "win"], w=[f"bk{bi}"])
                    S.op("act", lambda e, bi=bi, dst=dst: e.activation(out=dst[:, p3, :], in_=banks[bi][:, :],
                                                                       func=AF.Gelu_apprx_tanh),
                         r=[f"bk{bi}"], w=[f"{key}{p3}"])

            def stageB(j, S):
                pa = j % 2
                xs = j % 3
                p3 = j % 3
                ti = j - 1
                x_t = xb[:, xs, :]
                if cut == 1:
                    dump(S, 0, vg[:, p3, :], [f"vg{p3}"], 512)
                    return
                for h in range(8):
                    half, m = h // 4, h % 4
                    sb_ = 4 + (h % 2)
                    pr0 = half * 64
                    S.op("pe", lambda e, m=m, pr0=pr0, sb_=sb_: e.matmul(
                        banks[sb_][:, 0:384], lhsT=qT[pr0:pr0 + 64, p3, m, :],
                        rhs=kT[pr0:pr0 + 64, j - 1:j + 2, :].rearrange("p a t -> p (a t)"), start=True, stop=True),
                        r=[f"qT{p3}", f"k{j-1}", f"k{j}", f"k{j+1}"], w=[f"bk{sb_}"])
                    S.op("dve", lambda e, h=h, sb_=sb_: e.scalar_tensor_tensor(
                        out=sall[:, h, :], in0=banks[sb_][:, 0:384], scalar=0.125, in1=bt[:, h, :],
                        op0=ALU.mult, op1=ALU.add),
                        r=[f"bk{sb_}", "bt"], w=[f"sall{h}"])
                sk = [f"sall{h}" for h in range(8)]
                if j == 1:
                    S.op("dve", lambda e: e.tensor_scalar(out=sall[:, :, 0:128], in0=sall[:, :, 0:128],
                                                          scalar1=pr[:, PROW["edge"]:PROW["edge"] + 1], scalar2=None,
                                                          op0=ALU.add), r=sk + ["pr"], w=sk)
                if j == NT:
                    S.op("dve", lambda e: e.tensor_scalar(out=sall[:, :, 256:384], in0=sall[:, :, 256:384],
                                                          scalar1=pr[:, PROW["edge"] + 1:PROW["edge"] + 2], scalar2=None,
                                                          op0=ALU.add), r=sk + ["pr"], w=sk)
                mx = st[:, pa, 8:16]
                nmx = st[:, pa, 16:24]
                rsum = st[:, pa, 24:32]
                S.op("dve", lambda e: e.tensor_reduce(out=mx, in_=sall[:, :, :], axis=AX.X, op=ALU.max),
                     r=sk, w=[f"mx{pa}"])
                sinkb = pr[:, PROW["sink"]:PROW["sink"] + 8]
                S.op("dve", lambda e: e.tensor_tensor(out=mx, in0=mx, in1=sinkb, op=ALU.max),
                     r=[f"mx{pa}", "pr"], w=[f"mx{pa}"])
                S.op("dve", lambda e: e.tensor_scalar(out=nmx, in0=mx, scalar1=-1.0, scalar2=None, op0=ALU.mult),
                     r=[f"mx{pa}"], w=[f"nmx{pa}"])
                S.op("pool", lambda e: e.memset(rsum, 0.0), w=[f"rsum{pa}"])
                for h in range(8):
                    S.op("act", lambda e, h=h: e.activation(out=pbf[:, h, :], in_=sall[:, h, :], func=AF.Exp,
                                                            bias=nmx[:, h:h + 1], scale=1.0,
                                                            accum_out=rsum[:, h:h + 1]),
                         r=[f"sall{h}", f"nmx{pa}"], w=[f"pbf{h}", f"rsum{pa}"])
                S.op("dve", lambda e: e.tensor_tensor(out=sm[:], in0=sinkb, in1=nmx, op=ALU.add),
                     r=["pr", f"nmx{pa}"], w=["sm"])
                S.op("act", lambda e: e.activation(out=sm[:], in_=sm[:], func=AF.Exp), r=["sm"], w=["sm"])
                S.op("dve", lambda e: e.tensor_tensor(out=sm[:], in0=sm[:], in1=rsum, op=ALU.add),
                     r=["sm", f"rsum{pa}"], w=["sm"])
                S.op("dve", lambda e: e.reciprocal(out=sm[:], in_=sm[:]), r=["sm"], w=["sm"])
                for hp in range(4):
                    bi = 6 + (hp % 2)
                    tb = bank_bf(bi)
                    for hh in range(2):
                        h = hp * 2 + hh
                        for a in range(3):
                            S.op("pe", lambda e, h=h, a=a, hh=hh, tb=tb: e.transpose(
                                out=tb[:, hh * 384 + a * 128: hh * 384 + (a + 1) * 128],
                                in_=pbf[:, h, a * 128:(a + 1) * 128], identity=idb[:]),
                                r=[f"pbf{h}", "idb"], w=[f"bk{bi}"])
                    eng = "act" if hp % 2 == 0 else "dve"
                    if eng == "act":
                        S.op("act", lambda e, hp=hp, tb=tb: e.copy(
                            out=pT[:, hp * 2:hp * 2 + 2, :], in_=tb[:, 0:768].rearrange("p (h c) -> p h c", h=2)),
                            r=[f"bk{bi}"], w=[f"pT{hp}"])
                    else:
                        S.op("dve", lambda e, hp=hp, tb=tb: e.tensor_copy(
                            out=pT[:, hp * 2:hp * 2 + 2, :], in_=tb[:, 0:768].rearrange("p (h c) -> p h c", h=2)),
                            r=[f"bk{bi}"], w=[f"pT{hp}"])
                for h in range(8):
                    kvh = h // 4
                    for a in range(3):
                        S.op("pe", lambda e, h=h, a=a, kvh=kvh: e.matmul(
                            banks[4][:, h * 64:(h + 1) * 64], lhsT=pT[:, h, a * 128:(a + 1) * 128],
                            rhs=vv[:, j - 1 + a, kvh * 64:(kvh + 1) * 64], start=(a == 0), stop=(a == 2)),
                            r=[f"pT{h // 2}", f"v{j - 1 + a}"], w=["bk4"])
                S.op("dve", lambda e: e.tensor_tensor(
                    out=attn[:, :].rearrange("p (h d) -> p h d", h=8),
                    in0=banks[4][:, :].rearrange("p (h d) -> p h d", h=8),
                    in1=bcast(sm[:, :].unsqueeze(2), [128, 8, 64]), op=ALU.mult),
                    r=["bk4", "sm"], w=["attn"])
                if cut == 2:
                    dump(S, 0, attn[:, :], ["attn"], 512)
                    return
                sa = st[:, pa, 2:3]
                ra = st[:, pa, 3:4]
                S.op("pool", lambda e: e.memset(sa, 0.0), w=[f"sa{pa}"])
                S.op("act", lambda e: e.activation(out=junkB[:, 0:512], in_=attn[:], func=AF.Square, accum_out=sa),
                     r=["attn"], w=["junkB", f"sa{pa}"])
                rstd_ops(S, sa, ra, 512, [f"sa{pa}"], [f"ra{pa}"])
                S.op("dve", lambda e: e.scalar_tensor_tensor(
                    out=mix[:, 0:512], in0=attn[:], scalar=ra, in1=pr[:, PROW["g_attn"]:PROW["g_attn"] + 512],
                    op0=ALU.mult, op1=ALU.mult), r=["attn", f"ra{pa}", "pr"], w=["mixa"])
                s1_ = st[:, pa, 4:5]
                s2_ = st[:, pa, 5:6]
                mu = st[:, pa, 6:7]
                S.op("dve", lambda e: e.tensor_reduce(out=s1_, in_=vg[:, p3, :], axis=AX.X, op=ALU.add),
                     r=[f"vg{p3}"], w=[f"s1{pa}"])
                S.op("dve", lambda e: e.tensor_scalar(out=mu, in0=s1_, scalar1=-1.0 / 512, scalar2=None, op0=ALU.mult),
                     r=[f"s1{pa}"], w=[f"mu{pa}"])
                S.op("dve", lambda e: e.tensor_scalar(out=gmt[:], in0=vg[:, p3, :], scalar1=mu, scalar2=None,
                                                      op0=ALU.add), r=[f"vg{p3}", f"mu{pa}"], w=["gmt"])
                S.op("pool", lambda e: e.memset(s2_, 0.0), w=[f"s2{pa}"])
                S.op("act", lambda e: e.activation(out=junkB[:, 0:512], in_=gmt[:], func=AF.Square, accum_out=s2_),
                     r=["gmt"], w=["junkB", f"s2{pa}"])
                rstd_ops(S, s2_, s2_, 512, [f"s2{pa}"], [f"s2{pa}"])
                S.op("dve", lambda e: e.scalar_tensor_tensor(
                    out=gmt[:], in0=gmt[:], scalar=s2_, in1=pr[:, PROW["ln_g"]:PROW["ln_g"] + 512],
                    op0=ALU.mult, op1=ALU.mult), r=["gmt", f"s2{pa}", "pr"], w=["gmt"])
                S.op("dve", lambda e: e.tensor_tensor(out=vn[:], in0=gmt[:], in1=pr[:, PROW["ln_b"]:PROW["ln_b"] + 512],
                                                      op=ALU.add), r=["gmt", "pr"], w=["vn"])
                for g in range(8):
                    S.op("pe", lambda e, g=g: e.matmul(banks[5][:, g * 64:(g + 1) * 64], lhsT=wsb[:, g, :],
                                                       rhs=vn[:, g * 64:(g + 1) * 64], start=True, stop=True),
                         r=["wsb", "vn"], w=["bk5"])
                S.op("dve", lambda e: e.tensor_tensor(
                    out=gmt[:, :].rearrange("p (g c) -> p g c", g=8),
                    in0=banks[5][:, :].rearrange("p (g c) -> p g c", g=8),
                    in1=bcast(bsb[:, :].unsqueeze(2), [128, 8, 64]), op=ALU.add),
                    r=["bk5", "bsb"], w=["gmt"])
                S.op("dve", lambda e: e.tensor_tensor(out=gmt[:], in0=gmt[:], in1=ug[:, p3, :], op=ALU.mult),
                     r=["gmt", f"ug{p3}"], w=["gmt"])
                S.op("pool", lambda e: e.memset(s1_, 0.0), w=[f"s1{pa}"])
                S.op("act", lambda e: e.activation(out=junkB[:, 0:512], in_=gmt[:], func=AF.Square, accum_out=s1_),
                     r=["gmt"], w=["junkB", f"s1{pa}"])
                rstd_ops(S, s1_, s1_, 512, [f"s1{pa}"], [f"s1{pa}"])
                S.op("dve", lambda e: e.scalar_tensor_tensor(
                    out=mix[:, 512:1024], in0=gmt[:], scalar=s1_, in1=pr[:, PROW["g_gmlp"]:PROW["g_gmlp"] + 512],
                    op0=ALU.mult, op1=ALU.mult), r=["gmt", f"s1{pa}", "pr"], w=["mixg"])
                if cut == 3:
                    dump(S, 0, gmt[:, :], ["gmt"], 512)
                    return
                tb = bank_bf(6)
                for kc in range(8):
                    S.op("pe", lambda e, kc=kc: e.transpose(out=tb[:, kc * 128:(kc + 1) * 128],
                                                            in_=mix[:, kc * 128:(kc + 1) * 128], identity=idb[:]),
                         r=["mixa", "mixg", "idb"], w=["bk6"])
                S.op("act", lambda e: e.copy(out=mixT[:, :, :], in_=tb[:, 0:1024].rearrange("p (k t) -> p k t", k=8)),
                     r=["bk6"], w=["mixT"])
                so = st[:, pa, 2:3]
                so2 = st[:, pa, 3:4]
                S.op("pool", lambda e: e.memset(st[:, pa, 2:4], 0.0), w=[f"sa{pa}", f"ra{pa}"])
                for hf in range(2):
                    for kc in range(8):
                        S.op("pe", lambda e, kc=kc, hf=hf: e.matmul(
                            banks[4 + hf][:, :], lhsT=mixT[:, kc, :], rhs=wout[:, kc, hf * 512:(hf + 1) * 512],
                            start=(kc == 0), stop=(kc == 7)), r=["mixT", "wout"], w=[f"bk{4 + hf}"])
                    S.op("act", lambda e, hf=hf: e.activation(
                        out=junkB[:, 0:512], in_=banks[4 + hf][:, :], func=AF.Square,
                        accum_out=st[:, pa, 2 + hf:3 + hf]),
                        r=[f"bk{4 + hf}"], w=["junkB", f"sa{pa}" if hf == 0 else f"ra{pa}"])
                S.op("dve", lambda e: e.tensor_tensor(out=so, in0=so, in1=so2, op=ALU.add),
                     r=[f"sa{pa}", f"ra{pa}"], w=[f"sa{pa}"])
                rstd_ops(S, so, so, D, [f"sa{pa}"], [f"sa{pa}"])
                for hf in range(2):
                    sl = slice(hf * 512, (hf + 1) * 512)
                    S.op("dve", lambda e, hf=hf, sl=sl: e.scalar_tensor_tensor(
                        out=tmpB[:, sl], in0=banks[4 + hf][:, :], scalar=so, in1=mod[:, D * 2 + hf * 512:D * 2 + (hf + 1) * 512],
                        op0=ALU.mult, op1=ALU.mult), r=[f"bk{4 + hf}", f"sa{pa}", "mod2"], w=["tmpB"])
                S.op("dve", lambda e: e.tensor_tensor(out=x_t, in0=x_t, in1=tmpB[:], op=ALU.add),
                     r=[f"x{xs}", "tmpB"], w=[f"x{xs}"])
                if stage == 1:
                    S.dma("sp", "st1", lambda e: e.dma_start(out=out[ti, :, :], in_=x_t), r=[f"x{xs}"], w=["outd"])
                else:
                    S.dma("sp", "st1", lambda e: e.dma_start(out=x1d[ti, :, :], in_=x_t), r=[f"x{xs}"], w=["x1d"])
                s3 = st[:, pa, 4:5]
                S.op("pool", lambda e: e.memset(s3, 0.0), w=[f"s1{pa}"])
                S.op("act", lambda e: e.activation(out=junkB[:], in_=x_t, func=AF.Square, accum_out=s3),
                     r=[f"x{xs}"], w=["junkB", f"s1{pa}"])
                rstd_ops(S, s3, s3, D, [f"s1{pa}"], [f"s1{pa}"])
                S.op("dve", lambda e: e.scalar_tensor_tensor(out=h2f[:], in0=x_t, scalar=s3, in1=mod[:, GS2],
                                                             op0=ALU.mult, op1=ALU.mult),
                     r=[f"x{xs}", f"s1{pa}", "mod4"], w=["h2f"])
                S.op("dve", lambda e: e.tensor_tensor(out=h2f[:], in0=h2f[:], in1=mod[:, SH2], op=ALU.add),
                     r=["h2f", "mod3"], w=["h2f"])
                if stage >= 4:
                    S.op("act", lambda e: e.copy(out=mix[:], in_=h2f[:]), r=["h2f"], w=["mixa", "mixg"])
                    S.dma("sp", "h2st", lambda e: e.dma_start(out=h2loc[ti * 128:(ti + 1) * 128, :], in_=mix[:]),
                          r=["mixa", "mixg"], w=["h2loc"])
                for hf in range(2):
                    for kk in range(4):
                        kc = hf * 4 + kk
                        S.op("pe", lambda e, kc=kc, kk=kk, hf=hf: e.transpose(
                            out=banks[6 + hf][:, kk * 128:(kk + 1) * 128], in_=h2f[:, kc * 128:(kc + 1) * 128],
                            identity=idf[:]), r=["h2f", "idf"], w=[f"bk{6 + hf}"])
                    S.op("act", lambda e, hf=hf: e.copy(
                        out=h2Tf[:, hf * 4:(hf + 1) * 4, :], in_=banks[6 + hf][:, :].rearrange("p (k t) -> p k t", k=4)),
                        r=[f"bk{6 + hf}"], w=[f"h2Tf{hf}"])
                    if stage == 3:
                        S.dma("sp", "h2st", lambda e, hf=hf: e.dma_start(
                            out=h2Td[:, hf * 4:(hf + 1) * 4, ti * 128:(ti + 1) * 128], in_=h2Tf[:, hf * 4:(hf + 1) * 4, :]),
                            r=[f"h2Tf{hf}"], w=["h2Td"])
                for kc in range(8):
                    S.op("pe", lambda e, kc=kc: e.matmul(banks[4][:, 0:NE], lhsT=h2Tf[:, kc, :], rhs=wrs[:, kc, :],
                                                         start=(kc == 0), stop=(kc == 7)),
                         r=["h2Tf0", "h2Tf1", "wrs"], w=["bk4"])
                lm = st[:, pa, 6:7]
                ls = st[:, pa, 7:8]
                S.op("dve", lambda e: e.tensor_reduce(out=lm, in_=banks[4][:, 0:NE], axis=AX.X, op=ALU.max),
                     r=["bk4"], w=[f"mu{pa}"])
                S.op("dve", lambda e: e.tensor_scalar(out=lm, in0=lm, scalar1=-1.0, scalar2=None, op0=ALU.mult),
                     r=[f"mu{pa}"], w=[f"mu{pa}"])
                S.op("pool", lambda e: e.memset(ls, 0.0), w=[f"ls{pa}"])
                S.op("act", lambda e: e.activation(out=affo[:, ti, :], in_=banks[4][:, 0:NE], func=AF.Exp,
                                                   bias=lm, scale=1.0, accum_out=ls),
                     r=["bk4", f"mu{pa}"], w=["affo", f"ls{pa}"])
                S.op("dve", lambda e: e.reciprocal(out=ls, in_=ls), r=[f"ls{pa}"], w=[f"ls{pa}"])
                S.op("dve", lambda e: e.tensor_scalar(out=affo[:, ti, :], in0=affo[:, ti, :], scalar1=ls, scalar2=None,
                                                      op0=ALU.mult), r=["affo", f"ls{pa}"], w=["affo"])
                S.op("pe", lambda e: e.transpose(out=banks[4][0:NE, 128:256], in_=affo[:, ti, :], identity=idf[:]),
                     r=["affo", "idf"], w=["bk4"])
                S.op("act", lambda e: e.copy(out=affT[:, ti * 128:(ti + 1) * 128], in_=banks[4][0:NE, 128:256]),
                     r=["bk4"], w=["affT"])

            class Rec:
                def __init__(self):
                    self.calls = []

                def op(self, *a, **k):
                    self.calls.append(("op", a, k))

                def dma(self, *a, **k):
                    self.calls.append(("dma", a, k))

            def replay(calls):
                for kind, a, k in calls:
                    getattr(S, kind)(*a, **k)

            def merge(ca, cb):
                outl = []
                na, nb = len(ca), len(cb)
                ia_ = ib_ = 0
                while ia_ < na or ib_ < nb:
                    if ib_ >= nb or (ia_ < na and ia_ * nb <= ib_ * na):
                        outl.append(ca[ia_]); ia_ += 1
                    else:
                        outl.append(cb[ib_]); ib_ += 1
                return outl

            for j0 in range(3):
                r0 = Rec(); stageA(j0, r0); replay(r0.calls)
            for j in range(1, (NT if cut >= 90 else 1) + 1):
                ra, rb = Rec(), Rec()
                if j + 2 <= NT + 1:
                    stageA(j + 2, ra)
                stageB(j, rb)
                replay(merge(ra.calls, rb.calls))
            fw = []
            if stage == 1:
                fw = [("c_st1", S.chan["st1"] * 16)]
            else:
                S.dma("sp", "ag", lambda e: e.dma_start(out=agi[:, :], in_=affT[:, :]), r=["affT"], w=["agi"])
                fw = [("c_st1", S.chan["st1"] * 16), ("c_ag", 16), ("c_h2st", S.chan["h2st"] * 16)]
            sems = {n: gs.enter_context(nc.semaphore(f"b1_{n}")) for n in S.sem_names()}
            S.emit(sems, final_waits=fw)
        if stage == 1:
            return nc

        def moe_expert_sharded():
            with ExitStack() as s3:
                S = Sched(nc, "b3")
                xs = SB("xs", [128, 2, D], BF16, s3)
                xsT = SB("xsT", [128, 8, CAP], BF16, s3)
                hid = SB("hid", [128, NFC, CAP], BF16, s3)
                wdb = SB("wdb", [128, NFC, D], BF16, s3)
                wgs = SB("wgs", [128, 2, 8, 128], F32, s3)
                wus = SB("wus", [128, 2, 8, 128], F32, s3)
                wgb = SB("wgb", [128, 2, 8, 128], BF16, s3)
                wub = SB("wub", [128, 2, 8, 128], BF16, s3)
                wds = SB("wds", [128, 2, D], F32, s3)
                sg = SB("sg", [128, 2, 512], F32, s3)
                orow = SB("orow", [128, 2, D], BF16, s3)
                def coll_pair(lpp):
                    S.coll("ago", lambda e, lpp=lpp: e.collective_compute(
                        "AllGather", ALU.bypass, replica_groups=[list(range(8))],
                        ins=[outloc[lpp * CAP:(lpp + 1) * CAP, :]], outs=[outall[lpp * 8 * CAP:(lpp + 1) * 8 * CAP, :]]),
                        r=[f"outloc{lpp}"], w=["outall"])
                for lp in range(4):
                    el = lp // 2
                    for sc in range(8):
                        xb_ = sc % 2
                        col = lp * 8 + sc
                        S.dma("pool", f"g{xb_}", lambda e, xb_=xb_, col=col: e.indirect_dma_start(
                            out=xs[:, xb_, :], out_offset=None, in_=h2all[:, :],
                            in_offset=bass.IndirectOffsetOnAxis(ap=lidx[:, col:col + 1], axis=0)),
                            r=["lidx"], w=[f"xs{xb_}"])
                        tb = bank_bf(xb_)
                        for kc in range(8):
                            S.op("pe", lambda e, kc=kc, xb_=xb_, tb=tb: e.transpose(
                                out=tb[:, kc * 128:(kc + 1) * 128], in_=xs[:, xb_, kc * 128:(kc + 1) * 128], identity=idb[:]),
                                r=[f"xs{xb_}", "idb"], w=[f"bk{xb_}"])
                        if sc % 2 == 0:
                            S.op("act", lambda e, sc=sc, tb=tb: e.copy(
                                out=xsT[:, :, sc * 128:(sc + 1) * 128], in_=tb[:, 0:1024].rearrange("p (k t) -> p k t", k=8)),
                                r=[f"bk{xb_}"], w=["xsT"])
                        else:
                            S.op("dve", lambda e, sc=sc, tb=tb: e.tensor_copy(
                                out=xsT[:, :, sc * 128:(sc + 1) * 128], in_=tb[:, 0:1024].rearrange("p (k t) -> p k t", k=8)),
                                r=[f"bk{xb_}"], w=["xsT"])
                    if lp >= 1:
                        coll_pair(lp - 1)
                    for fc in range(NFC):
                        pa = fc % 2
                        S.dma("sp", f"wg{pa}", lambda e, el=el, fc=fc, pa=pa: e.dma_start(out=wgs[:, pa], in_=wg[el, fc]),
                              w=[f"wgs{pa}"])
                        S.dma("sp", f"wu{pa}", lambda e, el=el, fc=fc, pa=pa: e.dma_start(out=wus[:, pa], in_=wu[el, fc]),
                              w=[f"wus{pa}"])
                        if lp % 2 == 0:
                            S.dma("sp", f"wd{pa}", lambda e, el=el, fc=fc, pa=pa: e.dma_start(out=wds[:, pa, :], in_=wd[el, fc]),
                                  w=[f"wds{pa}"])
                        S.op("act", lambda e, pa=pa: e.copy(out=wgb[:, pa], in_=wgs[:, pa]), r=[f"wgs{pa}"], w=[f"wgb{pa}"])
                        S.op("dve", lambda e, pa=pa: e.tensor_copy(out=wub[:, pa], in_=wus[:, pa]), r=[f"wus{pa}"], w=[f"wub{pa}"])
                        if lp % 2 == 0:
                            if pa == 0:
                                S.op("act", lambda e, fc=fc, pa=pa: e.copy(out=wdb[:, fc, :], in_=wds[:, pa, :]),
                                     r=[f"wds{pa}"], w=[f"wdb{fc}"])
                            else:
                                S.op("dve", lambda e, fc=fc, pa=pa: e.tensor_copy(out=wdb[:, fc, :], in_=wds[:, pa, :]),
                                     r=[f"wds{pa}"], w=[f"wdb{fc}"])
                        for nh in range(2):
                            ga, ub = 2 + 2 * nh, 3 + 2 * nh
                            for kc in range(8):
                                S.op("pe", lambda e, kc=kc, pa=pa, nh=nh, ga=ga: e.matmul(
                                    banks[ga][:, :], lhsT=wgb[:, pa, kc, :], rhs=xsT[:, kc, nh * 512:(nh + 1) * 512],
                                    start=(kc == 0), stop=(kc == 7)), r=[f"wgb{pa}", "xsT"], w=[f"bk{ga}"])
                            for kc in range(8):
                                S.op("pe", lambda e, kc=kc, pa=pa, nh=nh, ub=ub: e.matmul(
                                    banks[ub][:, :], lhsT=wub[:, pa, kc, :], rhs=xsT[:, kc, nh * 512:(nh + 1) * 512],
                                    start=(kc == 0), stop=(kc == 7)), r=[f"wub{pa}", "xsT"], w=[f"bk{ub}"])
                            S.op("act", lambda e, nh=nh, ga=ga: e.activation(out=sg[:, nh, :], in_=banks[ga][:, :], func=AF.Silu),
                                 r=[f"bk{ga}"], w=[f"sg{nh}"])
                            S.op("dve", lambda e, nh=nh, ub=ub, fc=fc: e.tensor_tensor(
                                out=hid[:, fc, nh * 512:(nh + 1) * 512], in0=sg[:, nh, :], in1=banks[ub][:, :], op=ALU.mult),
                                r=[f"sg{nh}", f"bk{ub}"], w=["hid"])
                    for tt in range(8):
                        ob_ = tt % 2
                        for dh in range(2):
                            obk = 6 + dh
                            for fc in range(NFC):
                                S.op("pe", lambda e, fc=fc, tt=tt, dh=dh, obk=obk: e.matmul(
                                    banks[obk][:, :], lhsT=hid[:, fc, tt * 128:(tt + 1) * 128],
                                    rhs=wdb[:, fc, dh * 512:(dh + 1) * 512], start=(fc == 0), stop=(fc == NFC - 1)),
                                    r=["hid", f"wdb{fc}"], w=[f"bk{obk}"])
                            if dh == 0:
                                S.op("act", lambda e, ob_=ob_, obk=obk: e.copy(out=orow[:, ob_, 0:512], in_=banks[obk][:, :]),
                                     r=[f"bk{obk}"], w=[f"orow{ob_}"])
                            else:
                                S.op("dve", lambda e, ob_=ob_, obk=obk: e.tensor_copy(out=orow[:, ob_, 512:1024], in_=banks[obk][:, :]),
                                     r=[f"bk{obk}"], w=[f"orow{ob_}"])
                        S.dma("sp", f"os{ob_}", lambda e, lp=lp, tt=tt, ob_=ob_: e.dma_start(
                            out=outloc[lp * CAP + tt * 128: lp * CAP + (tt + 1) * 128, :], in_=orow[:, ob_, :]),
                            r=[f"orow{ob_}"], w=[f"outloc{lp}"])
                coll_pair(3)
                sems = {n: gs.enter_context(nc.semaphore(f"b3_{n}")) for n in S.sem_names()}
                S.emit(sems, final_waits=[(f"c_os{i}", S.chan[f"os{i}"] * 16) for i in range(2)] + [("c_ago", 4)])
            with ExitStack() as s4:
                S = Sched(nc, "b4")
                grow = SB("grow", [128, 4, D], BF16, s4)
                yac = SB("yac", [128, 2, D], F32, s4)
                x1t = SB("x1t", [128, 2, D], F32, s4)
                jk = SB("jk", [128, D], BF16, s4)
                st3 = SB("st3", [128, 4], F32, s4)
                epsc3 = pr[:, PROW["eps"]:PROW["eps"] + 1]
                breg = {}

                def bchk(e):
                    if "r" not in breg:
                        breg["r"] = e.to_reg(8 * 4 * CAP - 1)
                    return breg["r"]
                S.op("dve", lambda e: e.memset(grow[:], 0.0), w=[f"grow{i}" for i in range(4)])
                for ti in range(NT):
                    pa = ti % 2
                    S.dma("sp", f"x1l{pa}", lambda e, ti=ti, pa=pa: e.dma_start(out=x1t[:, pa, :], in_=x1d[ti, :, :]),
                          w=[f"x1t{pa}"])
                    S.op("dve", lambda e, pa=pa: e.memset(yac[:, pa, :], 0.0), w=[f"yac{pa}"])
                    for ex in range(NE):
                        gb = ex % 4
                        col = ti * NE + ex
                        S.dma("pool", f"gg{gb}", lambda e, gb=gb, col=col: e.indirect_dma_start(
                            out=grow[:, gb, :], out_offset=None, in_=outall[:, :],
                            in_offset=bass.IndirectOffsetOnAxis(ap=cidx[:, col:col + 1], axis=0),
                            bounds_check=bchk(e), oob_is_err=False), r=["cidx"], w=[f"grow{gb}"])
                        S.op("dve", lambda e, gb=gb, pa=pa, ti=ti, ex=ex: e.scalar_tensor_tensor(
                            out=yac[:, pa, :], in0=grow[:, gb, :], scalar=gm_all[:, ti, ex:ex + 1], in1=yac[:, pa, :],
                            op0=ALU.mult, op1=ALU.add), r=[f"grow{gb}", "gm_all", f"yac{pa}"], w=[f"yac{pa}"])
                    sq = st3[:, pa:pa + 1]
                    S.op("pool", lambda e, sq=sq: e.memset(sq, 0.0), w=[f"sq{pa}"])
                    S.op("act", lambda e, pa=pa, sq=sq: e.activation(out=jk[:], in_=yac[:, pa, :], func=AF.Square, accum_out=sq),
                         r=[f"yac{pa}"], w=["jk", f"sq{pa}"])
                    S.op("act", lambda e, sq=sq: e.activation(out=sq, in_=sq, func=AF.Ln, scale=1.0 / D, bias=epsc3),
                         r=[f"sq{pa}"], w=[f"sq{pa}"])
                    S.op("act", lambda e, sq=sq: e.activation(out=sq, in_=sq, func=AF.Exp, scale=-0.5),
                         r=[f"sq{pa}"], w=[f"sq{pa}"])
                    S.op("dve", lambda e, pa=pa, sq=sq: e.scalar_tensor_tensor(
                        out=yac[:, pa, :], in0=yac[:, pa, :], scalar=sq, in1=mod[:, GG2], op0=ALU.mult, op1=ALU.mult),
                        r=[f"yac{pa}", f"sq{pa}"], w=[f"yac{pa}"])
                    S.op("dve", lambda e, pa=pa: e.tensor_tensor(out=x1t[:, pa, :], in0=x1t[:, pa, :], in1=yac[:, pa, :],
                                                                 op=ALU.add), r=[f"yac{pa}", f"x1t{pa}"], w=[f"x1t{pa}"])
                    S.dma("sp", "ost", lambda e, ti=ti, pa=pa: e.dma_start(out=out[ti, :, :], in_=x1t[:, pa, :]),
                          r=[f"x1t{pa}"], w=["outd"])
                sems = {n: gs.enter_context(nc.semaphore(f"b4_{n}")) for n in S.sem_names()}
                S.emit(sems, final_waits=[("c_ost", S.chan["ost"] * 16)])

        def routing_tail(S, s2, affA, msk, lo, selo_s, thr, mk2, lobc):
            ones_t = SB("ones_t", [128, NT * 128], F32, s2)
            cum = SB("cum", [128, NT * 128], F32, s2)
            selq_s = SB("selq_s", [128, 128], F32, s2)
            selp_s = SB("selp_s", [128, NE], F32, s2)
            iota_s = SB("iota_s", [128, CAP], F32, s2)
            tokf = SB("tokf", [128, 64, 2], F32, s2)
            tokb = SB("tokb", [128, 64, 2], BF16, s2)
            w2_s = SB("w2_s", [2, 1], F32, s2)
            cb_s = SB("cb_s", [128, NE], F32, s2)
            pb_s = SB("pb_s", [128, 32], F32, s2)
            offc = SB("offc", [128, 1], F32, s2)
            GTp = SB("GTp", [128, NT * NE], F32, s2)
            PTo = SB("PTo", [128, NT * NE], F32, s2)
            tq = SB("tq", [128, NT * NE], F32, s2)
            oh = SB("oh", [128, 2, CAP], BF16, s2)
            Ls = SB("Ls", [2, CAP], F32, s2)
            lf = SB("lf", [128, 32], F32, s2)
            for (dst, src, k) in ((selq_s, selq, "selq"), (selp_s, selpair, "selp"), (iota_s, iota_d, "iota"),
                                  (w2_s, w2_d, "w2"), (cb_s, cbase_d, "cb"), (pb_s, pbase_d, "pb")):
                S.dma("sp", "l4", group=True, fn=lambda e, dst=dst, src=src: e.dma_start(out=dst[:], in_=src[:, :]), w=[k])
            S.dma("sp", "l4", group=True, fn=lambda e: e.dma_start(out=tokf[:], in_=tokid_d[:, :, :]), w=["tokf"])
            S.op("act", lambda e: e.copy(out=tokb[:], in_=tokf[:]), r=["tokf"], w=["tokb"])
            S.op("pool", lambda e: e.memset(ones_t[:], 1.0), w=["ones_t"])
            S.op("dve", lambda e: e.tensor_copy(out=lobc[:], in_=bcast(lo, [128, 128])), r=["bs"], w=["lobc"])
            S.op("pe", lambda e: e.matmul(banks[1][:, 0:NE], lhsT=lobc[:], rhs=selo_s[:], start=True, stop=True),
                 r=["lobc", "selo"], w=["bk1"])
            S.op("dve", lambda e: e.tensor_copy(out=thr[:], in_=banks[1][:, 0:NE]), r=["bk1"], w=["thr"])
            S.op("dve", lambda e: e.tensor_tensor(out=mk2[:], in0=affo[:], in1=bcast(thr[:, :].unsqueeze(1), [128, NT, NE]),
                                                  op=ALU.is_ge), r=["thr", "affo"], w=["mk2"])
            S.op("dve", lambda e: e.tensor_tensor(out=gm_all[:], in0=mk2[:], in1=affo[:], op=ALU.mult),
                 r=["mk2", "affo"], w=["gm_all"])
            S.op("dve", lambda e: e.tensor_scalar(out=msk[:], in0=affA[:], scalar1=lo, scalar2=None, op0=ALU.is_ge),
                 r=["affA", "bs"], w=["msk"])
            S.op("dve", lambda e: e.tensor_tensor_scan(out=cum[:], data0=ones_t[:], data1=msk[:], initial=0.0,
                                                       op0=ALU.mult, op1=ALU.add), r=["ones_t", "msk"], w=["cum"])
            S.op("pe", lambda e: e.matmul(banks[0][:, 0:1], lhsT=selq_s[:], rhs=cum[:, NT * 128 - 1:NT * 128],
                                          start=True, stop=True), r=["selq", "cum"], w=["bk0"])
            S.op("dve", lambda e: e.tensor_copy(out=offc[:], in_=banks[0][:, 0:1]), r=["bk0"], w=["offc"])
            S.op("dve", lambda e: e.scalar_tensor_tensor(out=cum[:], in0=cum[:], scalar=offc[:, 0:1], in1=msk[:],
                                                         op0=ALU.add, op1=ALU.mult), r=["cum", "offc", "msk"], w=["cum"])
            S.op("dve", lambda e: e.tensor_scalar(out=cum[:], in0=cum[:], scalar1=-1.0, scalar2=None, op0=ALU.add),
                 r=["cum"], w=["cum"])
            for (bk_i, sel_t, selk, dst, dk) in ((2, selp_s, "selp", GTp, "GTp"), (3, selo_s, "selo", PTo, "PTo")):
                for ch in range(NT):
                    S.op("pe", lambda e, ch=ch, bk_i=bk_i, sel_t=sel_t: e.matmul(
                        banks[bk_i][:, ch * NE:(ch + 1) * NE], lhsT=cum[:, ch * 128:(ch + 1) * 128], rhs=sel_t[:],
                        start=True, stop=True), r=["cum", selk], w=[f"bk{bk_i}"])
                S.op("dve", lambda e, bk_i=bk_i, dst=dst: e.tensor_copy(out=dst[:], in_=banks[bk_i][:, 0:NT * NE]),
                     r=[f"bk{bk_i}"], w=[dk])
            S.op("dve", lambda e: e.tensor_scalar(out=tq[:], in0=PTo[:], scalar1=0.0, scalar2=None, op0=ALU.is_lt),
                 r=["PTo"], w=["tq"])
            S.op("dve", lambda e: e.scalar_tensor_tensor(out=tq[:], in0=tq[:], scalar=1.0e6, in1=PTo[:], op0=ALU.mult,
                                                         op1=ALU.add), r=["tq", "PTo"], w=["tq"])
            S.op("dve", lambda e: e.tensor_tensor(out=tq[:, :].rearrange("p (c n) -> p c n", n=NE),
                                                  in0=tq[:, :].rearrange("p (c n) -> p c n", n=NE),
                                                  in1=bcast(cb_s[:, :].unsqueeze(1), [128, NT, NE]), op=ALU.add),
                 r=["tq", "cb"], w=["tq"])
            S.op("dve", lambda e: e.tensor_copy(out=cidx[:], in_=tq[:]), r=["tq"], w=["cidx"])
            for lp in range(4):
                pb0 = 4 + 2 * (lp % 2)
                for q in range(4):
                    n = lp * 4 + q
                    for ch in range(NT):
                        tile_i = q * NT + ch
                        ob = tile_i % 2
                        S.op("dve", lambda e, ch=ch, n=n, ob=ob: e.tensor_scalar(
                            out=oh[:, ob, :], in0=iota_s[:], scalar1=GTp[:, ch * NE + n:ch * NE + n + 1], scalar2=None,
                            op0=ALU.is_equal), r=["iota", "GTp"], w=[f"oh{ob}"])
                        for hf in range(2):
                            S.op("pe", lambda e, tile_i=tile_i, ob=ob, hf=hf, pb0=pb0: e.matmul(
                                banks[pb0 + hf][0:2, :], lhsT=tokb[:, tile_i, :], rhs=oh[:, ob, hf * 512:(hf + 1) * 512],
                                start=(tile_i == 0), stop=(tile_i == 63)), r=["tokb", f"oh{ob}"], w=[f"bk{pb0 + hf}"])
                for hf in range(2):
                    S.op("act", lambda e, hf=hf, pb0=pb0: e.copy(out=Ls[0:2, hf * 512:(hf + 1) * 512], in_=banks[pb0 + hf][0:2, :]),
                         r=[f"bk{pb0 + hf}"], w=["Ls"])
                for sc in range(8):
                    S.op("pe", lambda e, sc=sc, lp=lp: e.matmul(
                        banks[1][:, lp * 8 + sc:lp * 8 + sc + 1], lhsT=Ls[0:2, sc * 128:(sc + 1) * 128], rhs=w2_s[0:2, 0:1],
                        start=True, stop=True), r=["Ls", "w2"], w=["bk1"])
            S.op("dve", lambda e: e.tensor_tensor(out=lf[:], in0=banks[1][:, 0:32], in1=pb_s[:], op=ALU.add),
                 r=["bk1", "pb"], w=["lf"])
            S.op("dve", lambda e: e.tensor_copy(out=lidx[:], in_=lf[:]), r=["lf"], w=["lidx"])

        with ExitStack() as s2:
            S = Sched(nc, "b2")
            affA = SB("affA", [128, NT * 128], F32, s2)
            msk = SB("msk", [128, NT * 128], F32, s2)
            selm_s = SB("selm_s", [128, 128], F32, s2)
            selo_s = SB("selo_s", [128, NE], F32, s2)
            bs = SB("bs", [128, 8], F32, s2)
            lobc = SB("lobc", [128, 128], F32, s2)
            thr = SB("thr", [128, NE], F32, s2)
            mk2 = SB("mk2", [128, NT, NE], F32, s2)
            S.coll("agc", lambda e: e.collective_compute("AllGather", ALU.bypass, replica_groups=[list(range(8))],
                                                         ins=[agi.ap().opt()], outs=[ago.ap().opt()]), w=["ago"])
            if stage >= 4:
                S.coll("agh", lambda e: e.collective_compute("AllGather", ALU.bypass, replica_groups=[list(range(8))],
                                                             ins=[h2loc.ap().opt()], outs=[h2all.ap().opt()]), w=["h2all"])
            S.dma("sp", "l2", group=True, fn=lambda e: e.dma_start(out=selm_s[:], in_=selm[:, :]), w=["selm"])
            S.dma("sp", "l2", group=True, fn=lambda e: e.dma_start(out=selo_s[:], in_=selown[:, :]), w=["selo"])
            S.dma("sp", "l3", lambda e: e.dma_start(out=affA[:], in_=ago[:, :]), r=["ago"], w=["affA"])
            lo, hi, mid, cnt, ge, t1 = [bs[:, i:i + 1] for i in range(6)]
            S.op("dve", lambda e: e.memset(bs[:], 0.0), w=["bs"])
            S.op("dve", lambda e: e.memset(hi, 1.0), r=["bs"], w=["bs"])
            S.op("dve", lambda e: e.memset(mid, 0.5), r=["bs"], w=["bs"])
            for it in range(28):
                S.op("dve", lambda e: e.tensor_scalar(out=msk[:], in0=affA[:], scalar1=mid, scalar2=None, op0=ALU.is_ge),
                     r=["affA", "bs"], w=["msk"])
                S.op("dve", lambda e: e.tensor_reduce(out=cnt, in_=msk[:], axis=AX.X, op=ALU.add), r=["msk"], w=["bs"])
                S.op("pe", lambda e: e.matmul(banks[0][:, 0:1], lhsT=selm_s[:], rhs=cnt, start=True, stop=True),
                     r=["selm", "bs"], w=["bk0"])
                S.op("dve", lambda e: e.tensor_scalar(out=ge, in0=banks[0][:, 0:1], scalar1=float(CAP), scalar2=None,
                                                      op0=ALU.is_ge), r=["bk0"], w=["bs"])
                S.op("dve", lambda e: e.tensor_tensor(out=t1, in0=mid, in1=ge, op=ALU.mult), r=["bs"], w=["bs"])
                S.op("dve", lambda e: e.tensor_tensor(out=lo, in0=lo, in1=t1, op=ALU.max), r=["bs"], w=["bs"])
                S.op("dve", lambda e: e.scalar_tensor_tensor(out=t1, in0=ge, scalar=4.0, in1=mid, op0=ALU.mult, op1=ALU.add),
                     r=["bs"], w=["bs"])
                S.op("dve", lambda e: e.tensor_tensor(out=hi, in0=hi, in1=t1, op=ALU.min), r=["bs"], w=["bs"])
                S.op("dve", lambda e: e.tensor_scalar(out=t1, in0=hi, scalar1=0.5, scalar2=None, op0=ALU.mult),
                     r=["bs"], w=["bs"])
                S.op("dve", lambda e: e.scalar_tensor_tensor(out=mid, in0=lo, scalar=0.5, in1=t1, op0=ALU.mult, op1=ALU.add),
                     r=["bs"], w=["bs"])
            if stage == 3:
                S.op("dve", lambda e: e.tensor_copy(out=lobc[:], in_=bcast(lo, [128, 128])), r=["bs"], w=["lobc"])
                S.op("pe", lambda e: e.matmul(banks[1][:, 0:NE], lhsT=lobc[:], rhs=selo_s[:], start=True, stop=True),
                     r=["lobc", "selo"], w=["bk1"])
                S.op("dve", lambda e: e.tensor_copy(out=thr[:], in_=banks[1][:, 0:NE]), r=["bk1"], w=["thr"])
                S.op("dve", lambda e: e.tensor_tensor(out=mk2[:], in0=affo[:], in1=bcast(thr[:, :].unsqueeze(1), [128, NT, NE]),
                                                      op=ALU.is_ge), r=["thr", "affo"], w=["mk2"])
                S.op("dve", lambda e: e.tensor_tensor(out=gm_all[:], in0=mk2[:], in1=affo[:], op=ALU.mult),
                     r=["mk2", "affo"], w=["gm_all"])
            else:
                routing_tail(S, s2, affA, msk, lo, selo_s, thr, mk2, lobc)
            sems = {n: gs.enter_context(nc.semaphore(f"b2_{n}")) for n in S.sem_names()}
            S.emit(sems, final_waits=([("c_agh", 1)] if stage >= 4 else []))

        if stage >= 4:
            moe_expert_sharded()
            return nc
        with ExitStack() as s3:
            S = Sched(nc, "b3")
            HT = 512
            h2s = SB("h2s", [128, 8, 128], F32, s3)
            h2b = SB("h2b", [128, 8, HT], BF16, s3)
            hid = SB("hid", [128, NFC, HT], BF16, s3)
            wdb = SB("wdb", [128, NFC, D], BF16, s3)
            yac = SB("yac", [128, 4, D], F32, s3)
            wgs = SB("wgs", [128, 1, 8, 128], F32, s3)
            wus = SB("wus", [128, 1, 8, 128], F32, s3)
            wgb = SB("wgb", [128, 2, 8, 128], BF16, s3)
            wub = SB("wub", [128, 2, 8, 128], BF16, s3)
            wds = SB("wds", [128, 2, D], F32, s3)
            sg = SB("sg", [128, 2, 512], F32, s3)
            x1t = SB("x1t", [128, 2, D], F32, s3)
            jk = SB("jk", [128, D], BF16, s3)
            st3 = SB("st3", [128, 4], F32, s3)
            epsc3 = pr[:, PROW["eps"]:PROW["eps"] + 1]
            for half in range(4):
                for q4 in range(4):
                    S.dma("sp", "h2l", lambda e, half=half, q4=q4: e.dma_start(
                        out=h2s[:], in_=h2Td[:, :, half * HT + q4 * 128: half * HT + (q4 + 1) * 128]), w=["h2s"])
                    S.op("dve", lambda e, q4=q4: e.tensor_copy(out=h2b[:, :, q4 * 128:(q4 + 1) * 128], in_=h2s[:]),
                         r=["h2s"], w=["h2b"])
                S.op("dve", lambda e: e.memset(yac[:], 0.0), w=["yac"] + [f"yac{a}{b}" for a in range(4) for b in range(2)])
                for ex in range(NE):
                    for fc in range(NFC):
                        pa = fc % 2
                        S.dma("sp", "wg0", lambda e, ex=ex, fc=fc, pa=pa: e.dma_start(out=wgs[:, 0], in_=wg[ex, fc]),
                              w=["wgs"])
                        S.dma("sp", "wu0", lambda e, ex=ex, fc=fc, pa=pa: e.dma_start(out=wus[:, 0], in_=wu[ex, fc]),
                              w=["wus"])
                        S.op("act", lambda e, pa=pa: e.copy(out=wgb[:, pa], in_=wgs[:, 0]), r=["wgs"], w=[f"wgb{pa}"])
                        S.op("dve", lambda e, pa=pa: e.tensor_copy(out=wub[:, pa], in_=wus[:, 0]), r=["wus"], w=[f"wub{pa}"])
                        for nh in range(1):
                            ga, ub = 2 * (fc % 2), 2 * (fc % 2) + 1
                            for kc in range(8):
                                S.op("pe", lambda e, kc=kc, pa=pa, nh=nh, ga=ga: e.matmul(
                                    banks[ga][:, :], lhsT=wgb[:, pa, kc, :], rhs=h2b[:, kc, nh * 512:(nh + 1) * 512],
                                    start=(kc == 0), stop=(kc == 7)), r=[f"wgb{pa}", "h2b"], w=[f"bk{ga}"])
                            for kc in range(8):
                                S.op("pe", lambda e, kc=kc, pa=pa, nh=nh, ub=ub: e.matmul(
                                    banks[ub][:, :], lhsT=wub[:, pa, kc, :], rhs=h2b[:, kc, nh * 512:(nh + 1) * 512],
                                    start=(kc == 0), stop=(kc == 7)), r=[f"wub{pa}", "h2b"], w=[f"bk{ub}"])
                            S.op("act", lambda e, nh=nh, ga=ga: e.activation(out=sg[:, nh, :], in_=banks[ga][:, :], func=AF.Silu),
                                 r=[f"bk{ga}"], w=[f"sg{nh}"])
                            S.op("dve", lambda e, nh=nh, ub=ub, fc=fc: e.tensor_tensor(
                                out=hid[:, fc, nh * 512:(nh + 1) * 512], in0=sg[:, nh, :], in1=banks[ub][:, :], op=ALU.mult),
                                r=[f"sg{nh}", f"bk{ub}"], w=["hid"])
                    for fc in range(NFC):
                        pa = fc % 2
                        S.dma("sp", f"wd{pa}", lambda e, ex=ex, fc=fc, pa=pa: e.dma_start(out=wds[:, pa, :], in_=wd[ex, fc]),
                              w=[f"wds{pa}"])
                        if pa == 0:
                            S.op("act", lambda e, fc=fc, pa=pa: e.copy(out=wdb[:, fc, :], in_=wds[:, pa, :]),
                                 r=[f"wds{pa}"], w=["wdb"])
                        else:
                            S.op("dve", lambda e, fc=fc, pa=pa: e.tensor_copy(out=wdb[:, fc, :], in_=wds[:, pa, :]),
                                 r=[f"wds{pa}"], w=["wdb"])
                    for tt in range(4):
                        for dh in range(2):
                            ob = 4 + (tt * 2 + dh) % 4
                            for fc in range(NFC):
                                S.op("pe", lambda e, fc=fc, tt=tt, dh=dh, ob=ob: e.matmul(
                                    banks[ob][:, :], lhsT=hid[:, fc, tt * 128:(tt + 1) * 128],
                                    rhs=wdb[:, fc, dh * 512:(dh + 1) * 512], start=(fc == 0), stop=(fc == NFC - 1)),
                                    r=["hid", "wdb"], w=[f"bk{ob}"])
                            S.op("dve", lambda e, tt=tt, dh=dh, ob=ob, ex=ex, half=half: e.scalar_tensor_tensor(
                                out=yac[:, tt, dh * 512:(dh + 1) * 512], in0=banks[ob][:, :],
                                scalar=gm_all[:, half * 4 + tt, ex:ex + 1], in1=yac[:, tt, dh * 512:(dh + 1) * 512],
                                op0=ALU.mult, op1=ALU.add), r=[f"bk{ob}", "gm_all", f"yac{tt}{dh}"], w=[f"yac{tt}{dh}"])
                for tt in range(4):
                    ti = half * 4 + tt
                    pa = tt % 2
                    yk = [f"yac{tt}0", f"yac{tt}1"]
                    S.dma("sp", f"x1l{pa}", lambda e, ti=ti, pa=pa: e.dma_start(out=x1t[:, pa, :], in_=x1d[ti, :, :]),
                          w=[f"x1t{pa}"])
                    sq = st3[:, pa:pa + 1]
                    S.op("pool", lambda e, sq=sq: e.memset(sq, 0.0), w=[f"sq{pa}"])
                    S.op("act", lambda e, tt=tt, sq=sq: e.activation(out=jk[:], in_=yac[:, tt, :], func=AF.Square, accum_out=sq),
                         r=yk + ["yac"], w=["jk", f"sq{pa}"])
                    S.op("act", lambda e, sq=sq: e.activation(out=sq, in_=sq, func=AF.Ln, scale=1.0 / D, bias=epsc3),
                         r=[f"sq{pa}"], w=[f"sq{pa}"])
                    S.op("act", lambda e, sq=sq: e.activation(out=sq, in_=sq, func=AF.Exp, scale=-0.5),
                         r=[f"sq{pa}"], w=[f"sq{pa}"])
                    S.op("dve", lambda e, tt=tt, sq=sq: e.scalar_tensor_tensor(
                        out=yac[:, tt, :], in0=yac[:, tt, :], scalar=sq, in1=mod[:, GG2], op0=ALU.mult, op1=ALU.mult),
                        r=yk + [f"sq{pa}"], w=yk)
                    S.op("dve", lambda e, tt=tt, pa=pa: e.tensor_tensor(out=x1t[:, pa, :], in0=x1t[:, pa, :], in1=yac[:, tt, :],
                                                                        op=ALU.add), r=yk + [f"x1t{pa}"], w=[f"x1t{pa}"])
                    S.dma("sp", "ost", lambda e, ti=ti, pa=pa: e.dma_start(out=out[ti, :, :], in_=x1t[:, pa, :]),
                          r=[f"x1t{pa}"], w=["outd"])
            sems = {n: gs.enter_context(nc.semaphore(f"b3_{n}")) for n in S.sem_names()}
            S.emit(sems, final_waits=[("c_ost", S.chan["ost"] * 16)])
        return nc


def _bias_table():
    slopes = np.exp2(-8.0 * np.arange(1, 9, dtype=np.float32) / 8).astype(np.float32)
    r = np.arange(128)[:, None]
    cpos = np.arange(384)[None, :] - 128
    dist = np.abs(r - cpos)
    valid = dist <= 128
    tab = np.empty((128, 8, 384), np.float32)
    for h in range(8):
        tab[:, h, :] = np.where(valid, -slopes[h] * dist.astype(np.float32), np.float32(-1e30))
    return tab


def _qperm():
    cols = []
    for m in range(4):
        cols += list(range(m * 64, (m + 1) * 64))
        cols += list(range((4 + m) * 64, (5 + m) * 64))
    return np.array(cols + list(range(512, 1792)))


def _prep(inp, stage):
    f = lambda a: np.ascontiguousarray(np.asarray(a, dtype=np.float32))
    x = f(inp["x"]); c = f(inp["c"])
    w_in = f(inp["w_in"])[0][:, _qperm()].reshape(8, 128, 1792)
    w_ada = f(inp["w_ada"])[0].reshape(8, 128, 6 * D)
    b_ada = f(inp["b_ada"])[0].reshape(1, 6 * D)
    w_out = f(inp["w_out"])[0].reshape(8, 128, D)
    w_sT = np.ascontiguousarray(f(inp["w_s"])[0].transpose(2, 0, 1))
    b_sT = np.ascontiguousarray(f(inp["b_s"])[0].T)
    w_r = f(inp["w_router"])[0].reshape(8, 128, NE)
    shared = dict(w_ada=w_ada, b_ada=b_ada, w_in=np.ascontiguousarray(w_in), w_out=w_out,
                  biast=_bias_table(), w_sT=w_sT, b_sT=b_sT, w_r=w_r, ident=np.eye(128, dtype=np.float32))
    if stage == 3:
        shared["wg"] = np.ascontiguousarray(f(inp["w_gate"])[0].reshape(NE, 8, 128, NFC, 128).transpose(0, 3, 2, 1, 4))
        shared["wu"] = np.ascontiguousarray(f(inp["w_up"])[0].reshape(NE, 8, 128, NFC, 128).transpose(0, 3, 2, 1, 4))
        shared["wd"] = f(inp["w_down"])[0].reshape(NE, NFC, 128, D)
        pp = np.arange(128)
        shared["selm"] = ((pp[:, None] // 64 == pp[None, :] // 64) & (pp[:, None] % 16 == pp[None, :] % 16)).astype(np.float32)
    if stage >= 4:
        wg_all = f(inp["w_gate"])[0].reshape(NE, 8, 128, NFC, 128)
        wu_all = f(inp["w_up"])[0].reshape(NE, 8, 128, NFC, 128)
        wd_all = f(inp["w_down"])[0].reshape(NE, NFC, 128, D)
        pp = np.arange(128)
        same = (pp[:, None] // 64 == pp[None, :] // 64) & (pp[:, None] % 16 == pp[None, :] % 16)
        shared["selm"] = same.astype(np.float32)
        qq = (pp // 16) % 4
        shared["selq"] = (same & (qq[:, None] < qq[None, :])).astype(np.float32)
        shared["iota_d"] = np.ascontiguousarray(np.broadcast_to(np.arange(CAP, dtype=np.float32), (128, CAP)))
        tk = np.zeros((128, 64, 2), np.float32)
        tk[:, :, 0] = np.arange(128)[:, None]
        tk[:, :, 1] = np.arange(64)[None, :]
        shared["tokid_d"] = tk
        shared["w2_d"] = np.array([[1.0], [128.0]], np.float32)
    maps = []
    for cid in range(8):
        b, qd = cid // 4, cid % 4
        t0 = qd * 2048
        xin = np.zeros((NT + 2, 128, D), np.float32)
        lo, hi = t0 - 128, t0 + 2048 + 128
        lo_c, hi_c = max(lo, 0), min(hi, SEQ)
        xin.reshape(-1, D)[lo_c - lo: hi_c - lo] = x[b, lo_c:hi_c]
        prow = np.zeros((128, NPROW), np.float32)
        prowA = np.zeros((128, 4 * D), np.float32)
        for name, key in (("g_pre", "norm_pre_mix"), ("g_post", "norm_post_mix"), ("g_pre2", "norm_pre_ffn"),
                          ("g_post2", "norm_post_ffn")):
            prowA[:, PROWA[name]:PROWA[name] + D] = f(inp[key])[0][None, :]
        for name, key in (("ln_g", "sgu_ln_g"), ("ln_b", "sgu_ln_b"),
                          ("g_attn", "norm_out_attn"), ("g_gmlp", "norm_out_gmlp"), ("sink", "sink")):
            v = f(inp[key])[0]
            prow[:, PROW[name]:PROW[name] + v.shape[0]] = v[None, :]
        prow[:, PROW["edge"]] = -1e30 if qd == 0 else 0.0
        prow[:, PROW["edge"] + 1] = -1e30 if qd == 3 else 0.0
        prow[:, PROW["eps"]] = EPS
        m = dict(shared)
        if stage >= 4:
            m["wg"] = np.ascontiguousarray(wg_all[2 * cid:2 * cid + 2].transpose(0, 3, 2, 1, 4))
            m["wu"] = np.ascontiguousarray(wu_all[2 * cid:2 * cid + 2].transpose(0, 3, 2, 1, 4))
            m["wd"] = np.ascontiguousarray(wd_all[2 * cid:2 * cid + 2])
            sp_ = np.zeros((128, NE), np.float32)
            pbase = np.zeros((128, 32), np.float32)
            for lp in range(4):
                el, bb = lp // 2, lp % 2
                for q_ in range(4):
                    sp_[(bb * 4 + q_) * 16 + 2 * cid + el, lp * 4 + q_] = 1.0
                pbase[:, lp * 8:(lp + 1) * 8] = bb * SEQ
            m["selpair"] = sp_
            m["pbase_d"] = pbase
            ee = np.arange(NE)
            m["cbase_d"] = np.ascontiguousarray(np.broadcast_to(
                (((ee % 2) * 2 + b) * 8 * CAP + (ee // 2) * CAP).astype(np.float32), (128, NE)))
        if stage >= 3:
            so = np.zeros((128, NE), np.float32)
            so[cid * 16 + np.arange(16), np.arange(16)] = 1.0
            m["selown"] = so
        m.update(xin=xin, cvec=np.ascontiguousarray(c[b].reshape(8, 128).T), prow=prow, prowA=prowA)
        maps.append(m)
    return maps


_CACHE = {}


def kernel(**inputs):
    stage = STAGE
    if stage not in _CACHE:
        _CACHE[stage] = build_program(stage)
    nc = _CACHE[stage]
    maps = _prep(inputs, stage)
    res = run_bass_kernel_spmd(nc, maps, core_ids=list(range(8)))
    outs = [np.asarray(r["out"], dtype=np.float32).reshape(2048, D) for r in res.results]
    full = np.stack([np.concatenate(outs[0:4], 0), np.concatenate(outs[4:8], 0)], 0)
    return full.astype(np.float32)
```

```python
import numpy as np
import ml_dtypes
import concourse.bass as bass
import concourse.mybir as mybir
from concourse.bass_utils import run_bass_kernel_spmd

F32 = mybir.dt.float32
BF16 = mybir.dt.bfloat16
I32 = mybir.dt.int32
AF = mybir.ActivationFunctionType
ALU = mybir.AluOpType
AX = mybir.AxisListType

D = 1024
SEQ = 8192
NT = 16
NE = 16
FF = 2816
NFC = FF // 128
CAP = 1024
EPS = 1e-6
STAGE = 4


class Sched:
    ENGS = ("pe", "act", "dve", "pool", "sp")

    def __init__(self, nc, tag):
        self.nc = nc
        self.tag = tag
        self.q = {e: [] for e in self.ENGS}
        self.cnt = {e: 0 for e in self.ENGS}
        self.last_w = {}
        self.readers = {}
        self.chan = {}
        self.sems = {}

    def _deps(self, r, w):
        deps = set()
        for k in r:
            if k in self.last_w:
                deps.add(self.last_w[k])
        for k in w:
            if k in self.last_w:
                deps.add(self.last_w[k])
            deps |= self.readers.get(k, set())
        return deps

    def _commit(self, r, w, tk):
        for k in r:
            self.readers.setdefault(k, set()).add(tk)
        for k in w:
            self.last_w[k] = tk
            self.readers[k] = set()

    def op(self, eng, fn, r=(), w=()):
        r = list(r); w = list(w)
        w += [k for k in r if k.startswith("bk") and k not in w]
        deps = self._deps(r, w)
        self.cnt[eng] += 1
        tk = ("e_" + eng, self.cnt[eng])
        if eng == "pe":
            deps = {d for d in deps if d[0] != "e_pe"}
        self.q[eng].append((fn, deps, tk[0], 1))
        self._commit(r, w, tk)
        return tk

    def dma(self, eng, ch, fn, r=(), w=(), group=False):
        deps = self._deps(r, w)
        self.chan[ch] = self.chan.get(ch, 0) + 1
        tk = ("c_" + ch, -1 if group else self.chan[ch] * 16)
        self.q[eng].append((fn, deps, tk[0], 16))
        self._commit(r, w, tk)
        return tk

    def coll(self, ch, fn, r=(), w=()):
        deps = self._deps(r, w)
        self.chan[ch] = self.chan.get(ch, 0) + 1
        tk = ("c_" + ch, self.chan[ch])
        self.q["pool"].append((fn, deps, tk[0], 1))
        self._commit(r, w, tk)
        return tk

    def sem_names(self):
        names = ["e_" + e for e in self.ENGS if self.cnt[e] > 0]
        names += ["c_" + c for c in self.chan]
        return names

    def emit(self, sems, final_waits=()):
        nc = self.nc
        with nc.Block() as block:
            def mk(engname):
                def body(e):
                    waited = {}
                    for (fn, deps, semname, inc) in self.q[engname]:
                        deps = {(sn, (self.chan[sn[2:]] * 16 if val == -1 else val)) for (sn, val) in deps}
                        for (sn, val) in sorted(deps):
                            if waited.get(sn, 0) >= val:
                                continue
                            e.wait_ge(sems[sn], val)
                            waited[sn] = val
                        ins = fn(e)
                        if inc == 1 and semname.startswith("c_"):
                            ins.then_inc(sems[semname])
                        else:
                            ins.then_inc(sems[semname], inc)
                    if engname == "sp":
                        for (sn, val) in final_waits:
                            e.wait_ge(sems[sn], val)
                return body
            if self.q["pe"]:
                block.tensor(mk("pe"))
            if self.q["act"]:
                block.scalar(mk("act"))
            if self.q["dve"]:
                block.vector(mk("dve"))
            if self.q["pool"]:
                block.gpsimd(mk("pool"))
            if self.q["sp"] or final_waits:
                block.sync(mk("sp"))


def bcast(ap, shape):
    return ap.to_broadcast(shape)


PROWA = dict(g_pre=0, g_post=1024, g_pre2=2048, g_post2=3072)
PROW = dict(ln_g=0, ln_b=512, g_attn=1024, g_gmlp=1536, sink=2048, edge=2056, eps=2058)
NPROW = 2064


def build_program(stage=STAGE, cut=99):
    nc = bass.Bass("TRN2", target_bir_lowering=False)
    dt = nc.dram_tensor
    xin = dt("xin", [NT + 2, 128, D], F32, kind="ExternalInput").ap()
    cvec = dt("cvec", [128, 8], F32, kind="ExternalInput").ap()
    w_ada = dt("w_ada", [8, 128, 6 * D], F32, kind="ExternalInput").ap()
    b_ada = dt("b_ada", [1, 6 * D], F32, kind="ExternalInput").ap()
    w_in = dt("w_in", [8, 128, 1792], F32, kind="ExternalInput").ap()
    w_out = dt("w_out", [8, 128, D], F32, kind="ExternalInput").ap()
    prow = dt("prow", [128, NPROW], F32, kind="ExternalInput").ap()
    prowA = dt("prowA", [128, 4 * D], F32, kind="ExternalInput").ap()
    biast = dt("biast", [128, 8, 384], F32, kind="ExternalInput").ap()
    w_sT = dt("w_sT", [128, 8, 128], F32, kind="ExternalInput").ap()
    b_sT = dt("b_sT", [128, 8], F32, kind="ExternalInput").ap()
    w_r = dt("w_r", [8, 128, NE], F32, kind="ExternalInput").ap()
    ident = dt("ident", [128, 128], F32, kind="ExternalInput").ap()
    if stage == 3:
      wg = dt("wg", [NE, NFC, 128, 8, 128], F32, kind="ExternalInput").ap()
      wu = dt("wu", [NE, NFC, 128, 8, 128], F32, kind="ExternalInput").ap()
      wd = dt("wd", [NE, NFC, 128, D], F32, kind="ExternalInput").ap()
      selm = dt("selm", [128, 128], F32, kind="ExternalInput").ap()
      selown = dt("selown", [128, NE], F32, kind="ExternalInput").ap()
    if stage >= 4:
      wg = dt("wg", [2, NFC, 128, 8, 128], F32, kind="ExternalInput").ap()
      wu = dt("wu", [2, NFC, 128, 8, 128], F32, kind="ExternalInput").ap()
      wd = dt("wd", [2, NFC, 128, D], F32, kind="ExternalInput").ap()
      selm = dt("selm", [128, 128], F32, kind="ExternalInput").ap()
      selown = dt("selown", [128, NE], F32, kind="ExternalInput").ap()
      selq = dt("selq", [128, 128], F32, kind="ExternalInput").ap()
      selpair = dt("selpair", [128, NE], F32, kind="ExternalInput").ap()
      iota_d = dt("iota_d", [128, CAP], F32, kind="ExternalInput").ap()
      tokid_d = dt("tokid_d", [128, 64, 2], F32, kind="ExternalInput").ap()
      w2_d = dt("w2_d", [2, 1], F32, kind="ExternalInput").ap()
      cbase_d = dt("cbase_d", [128, NE], F32, kind="ExternalInput").ap()
      pbase_d = dt("pbase_d", [128, 32], F32, kind="ExternalInput").ap()
      h2loc = dt("h2loc", [NT * 128, D], BF16)
      h2all = dt("h2all", [8 * NT * 128, D], BF16)
      outloc = dt("outloc", [4 * CAP, D], BF16)
      outall = dt("outall", [8 * 4 * CAP, D], BF16)
    out = dt("out", [NT, 128, D], F32, kind="ExternalOutput").ap()
    x1d = dt("x1d", [NT, 128, D], F32)
    h2Td = dt("h2Td", [128, 8, NT * 128], F32)
    agi = dt("agi", [NE, NT * 128], F32)
    ago = dt("ago", [8 * NE, NT * 128], F32)

    from contextlib import ExitStack
    with ExitStack() as gs:
        def SB(name, shape, dtype=F32, stack=gs):
            return stack.enter_context(nc.sbuf_tensor(name, shape, dtype))

        def PS(name, shape, dtype=F32, stack=gs):
            return stack.enter_context(nc.psum_tensor(name, shape, dtype))

        mod = SB("mod", [128, 6 * D])
        pr = SB("pr", [128, NPROW])
        idf = SB("idf", [128, 128])
        idb = SB("idb", [128, 128], BF16)
        affo = SB("affo", [128, NT, NE])
        gm_all = SB("gm_all", [128, NT, NE])
        lidx = SB("lidx", [128, 32], I32)
        cidx = SB("cidx", [128, NT * NE], I32)
        banks = [PS(f"bank{i}", [128, 512]) for i in range(8)]

        def bank_bf(i):
            return banks[i][:].bitcast(BF16)

        SH1, GS1, GG1, SH2, GS2, GG2 = [slice(i * D, (i + 1) * D) for i in range(6)]

        with ExitStack() as s0:
            S = Sched(nc, "b0")
            cv = SB("cv", [128, 8], F32, s0)
            scb = SB("scb", [128, 8, 128], F32, s0)
            bada = SB("bada", [1, 2, 512], F32, s0)
            ones1 = SB("ones1", [1, 128], F32, s0)
            wab = SB("wab", [128, 2, 8, 512], F32, s0)
            prA = SB("prA", [128, 4 * D], F32, s0)
            S.dma("sp", "ld0", group=True, fn=lambda e: e.dma_start(out=cv[:], in_=cvec[:, :]), w=["cv"])
            S.dma("sp", "ld0", group=True, fn=lambda e: e.dma_start(out=prA[:], in_=prowA[:, :]), w=["prA"])
            S.dma("sp", "ld0", group=True, fn=lambda e: e.dma_start(out=pr[:], in_=prow[:, :]), w=["pr"])
            S.dma("sp", "ld0", group=True, fn=lambda e: e.dma_start(out=idf[:], in_=ident[:, :]), w=["idf"])
            S.op("dve", lambda e: e.tensor_copy(out=idb[:], in_=idf[:]), r=["idf"], w=["idb"])
            S.op("pool", lambda e: e.memset(ones1[:], 1.0), w=["ones1"])
            S.op("act", lambda e: e.activation(out=cv[:], in_=cv[:], func=AF.Silu), r=["cv"], w=["cv"])
            S.op("dve", lambda e: e.tensor_copy(out=scb[:], in_=bcast(cv[:, :].unsqueeze(2), [128, 8, 128])),
                 r=["cv"], w=["scb"])
            for nb in range(12):
                pa = nb % 2
                S.dma("sp", f"wa{pa}",
                      lambda e, nb=nb, pa=pa: e.dma_start(
                          out=wab[:, pa, :, :],
                          in_=w_ada[:, :, nb * 512:(nb + 1) * 512].rearrange("k p n -> p k n")),
                      w=[f"wab{pa}"])
                S.dma("sp", f"ba{pa}",
                      lambda e, nb=nb, pa=pa: e.dma_start(out=bada[0:1, pa, :], in_=b_ada[0:1, nb * 512:(nb + 1) * 512]),
                      w=[f"bada{pa}"])
                bk = banks[nb % 2]
                for kc in range(8):
                    S.op("pe", lambda e, kc=kc, pa=pa, bk=bk: e.matmul(
                        bk[:, :], lhsT=scb[:, kc, :], rhs=wab[:, pa, kc, :], start=(kc == 0), stop=False),
                        r=[f"wab{pa}", "scb"], w=[f"bk{nb % 2}"])
                S.op("pe", lambda e, pa=pa, bk=bk: e.matmul(
                    bk[:, :], lhsT=ones1[0:1, :], rhs=bada[0:1, pa, :], start=False, stop=True),
                    r=["ones1", f"bada{pa}"], w=[f"bk{nb % 2}"])
                S.op("act", lambda e, nb=nb, bk=bk: e.copy(out=mod[:, nb * 512:(nb + 1) * 512], in_=bk[:, :]),
                     r=[f"bk{nb % 2}"], w=[f"mod{nb // 2}"])

            def fold(mi, sl, gname, plus1):
                g = prA[:, PROWA[gname]:PROWA[gname] + D]
                if plus1:
                    S.op("dve", lambda e: e.scalar_tensor_tensor(out=mod[:, sl], in0=mod[:, sl], scalar=1.0, in1=g,
                                                                 op0=ALU.add, op1=ALU.mult),
                         r=[f"mod{mi}", "prA"], w=[f"mod{mi}"])
                else:
                    S.op("dve", lambda e: e.tensor_tensor(out=mod[:, sl], in0=mod[:, sl], in1=g, op=ALU.mult),
                         r=[f"mod{mi}", "prA"], w=[f"mod{mi}"])
            fold(1, GS1, "g_pre", True)
            fold(2, GG1, "g_post", False)
            fold(4, GS2, "g_pre2", True)
            fold(5, GG2, "g_post2", False)
            fw0 = []
            if stage == 0:
                for i in range(6):
                    S.dma("sp", "st0", lambda e, i=i: e.dma_start(out=out[i, :, :], in_=mod[:, i * D:(i + 1) * D]),
                          r=[f"mod{i}"], w=["outd"])
                fw0 = [("c_st0", 96)]
            sems = {n: gs.enter_context(nc.semaphore(f"b0_{n}")) for n in S.sem_names()}
            S.emit(sems, final_waits=fw0)
        if stage == 0:
            return nc

        with ExitStack() as s1:
            S = Sched(nc, "b1")
            win = SB("win", [128, 8, 1792], BF16, s1)
            wout = SB("wout", [128, 8, D], BF16, s1)
            wsb = SB("wsb", [128, 8, 128], BF16, s1)
            bsb = SB("bsb", [128, 8], F32, s1)
            wrs = SB("wrs", [128, 8, NE], F32, s1)
            bt = SB("bt", [128, 8, 384], F32, s1)
            xb = SB("xb", [128, 3, D], F32, s1)
            st = SB("st", [128, 2, 32], F32, s1)
            stA = SB("stA", [128, 3, 8], F32, s1)
            hbf = SB("hbf", [128, 1, D], BF16, s1)
            hT = SB("hT", [128, 2, 8, 128], BF16, s1)
            qT = SB("qT", [128, 3, 4, 128], BF16, s1)
            kT = SB("kT", [128, NT + 2, 128], BF16, s1)
            vv = SB("vv", [128, NT + 2, 128], BF16, s1)
            ug = SB("ug", [128, 3, 512], BF16, s1)
            vg = SB("vg", [128, 3, 512], F32, s1)
            sall = SB("sall", [128, 8, 384], F32, s1)
            pbf = SB("pbf", [128, 8, 384], BF16, s1)
            pT = SB("pT", [128, 8, 384], BF16, s1)
            attn = SB("attn", [128, 512], F32, s1)
            junkA = SB("junkA", [128, D], BF16, s1)
            junkB = SB("junkB", [128, D], BF16, s1)
            vn = SB("vn", [128, 512], BF16, s1)
            gmt = SB("gmt", [128, 512], F32, s1)
            mix = SB("mix", [128, D], BF16, s1)
            mixT = SB("mixT", [128, 8, 128], BF16, s1)
            tmpA = SB("tmpA", [128, D], F32, s1)
            tmpB = SB("tmpB", [128, D], F32, s1)
            h2f = SB("h2f", [128, D], F32, s1)
            h2Tf = SB("h2Tf", [128, 8, 128], F32, s1)
            affT = SB("affT", [NE, NT * 128], F32, s1)
            sm = SB("sm", [128, 8], F32, s1)
            import os
            LM = int(os.environ.get("LM", "31"))
            S.dma("sp", "ld0", group=True, fn=lambda e: e.dma_start(out=bt[:], in_=biast[:, :, :]), w=["bt"])
            S.dma("sp", "ld0", group=True, fn=lambda e: e.dma_start(out=bsb[:], in_=b_sT[:, :]), w=["bsb"])
            if LM & 2:
                S.dma("sp", "ld0", group=True, fn=lambda e: e.dma_start(out=wrs[:], in_=w_r.rearrange("k p n -> p k n")), w=["wrs"])
            wstg = SB("wstg", [128, 2, 1024], F32, s1)
            S.dma("sp", "ws1", lambda e: e.dma_start(out=wstg[:, 1, 0:1024], in_=w_sT.rearrange("p g t -> p (g t)")),
                  w=["wstg1"])
            S.op("act", lambda e: e.copy(out=wsb[:, :, :].rearrange("p g t -> p (g t)"), in_=wstg[:, 1, 0:1024]),
                 r=["wstg1"], w=["wsb"])
            for kc in range(8):
                for hh in range(2):
                    sp_ = hh
                    c0 = hh * 896
                    S.dma("sp", f"ws{sp_}", lambda e, kc=kc, sp_=sp_, c0=c0: e.dma_start(
                        out=wstg[:, sp_, 0:896], in_=w_in[kc, :, c0:c0 + 896]), w=[f"wstg{sp_}"])
                    if hh:
                        S.op("act", lambda e, kc=kc, sp_=sp_, c0=c0: e.copy(out=win[:, kc, c0:c0 + 896], in_=wstg[:, sp_, 0:896]),
                             r=[f"wstg{sp_}"], w=["win"])
                    else:
                        S.op("dve", lambda e, kc=kc, sp_=sp_, c0=c0: e.tensor_copy(out=win[:, kc, c0:c0 + 896], in_=wstg[:, sp_, 0:896]),
                             r=[f"wstg{sp_}"], w=["win"])
            for kc in range(8):
                sp_ = kc % 2
                S.dma("sp", f"ws{sp_}", lambda e, kc=kc, sp_=sp_: e.dma_start(out=wstg[:, sp_, 0:D], in_=w_out[kc, :, :]),
                      w=[f"wstg{sp_}"])
                S.op("act" if kc % 2 else "dve", (lambda e, kc=kc, sp_=sp_: e.copy(out=wout[:, kc, :], in_=wstg[:, sp_, 0:D])) if kc % 2 else
                     (lambda e, kc=kc, sp_=sp_: e.tensor_copy(out=wout[:, kc, :], in_=wstg[:, sp_, 0:D])),
                     r=[f"wstg{sp_}"], w=["wout"])
            epsc = pr[:, PROW["eps"]:PROW["eps"] + 1]

            def rstd_ops(S, ssq_ap, out_ap, n, keys_r, keys_w):
                S.op("act", lambda e: e.activation(out=out_ap, in_=ssq_ap, func=AF.Ln, scale=1.0 / n, bias=epsc),
                     r=keys_r + ["pr"], w=keys_w)
                S.op("act", lambda e: e.activation(out=out_ap, in_=out_ap, func=AF.Exp, scale=-0.5),
                     r=keys_w, w=keys_w)

            def dump(S, slot, ap, keys, n):
                S.dma("sp", "st1", lambda e: e.dma_start(out=out[slot, :, 0:n], in_=ap), r=keys, w=["outd"])

            def stageA(j, S):
                pa = j % 2
                xs = j % 3
                p3 = j % 3
                halo = (j == 0 or j == NT + 1)
                x_t = xb[:, xs, :]
                if cut == -1:
                    if j == 0:
                        dump(S, 0, bt[:, 0, :], ["bt"], 384)
                        dump(S, 1, mod[:, 0:384], [], 384)
                        dump(S, 2, pr[:, 0:384], [], 384)
                        dump(S, 3, bt[:, 1, :], ["bt"], 384)
                    return
                S.dma("sp", f"x{xs}", lambda e: e.dma_start(out=x_t, in_=xin[j, :, :]), w=[f"x{xs}"])
                ssq = stA[:, p3, 0:1]
                rs = stA[:, p3, 1:2]
                S.op("pool", lambda e: e.memset(ssq, 0.0), w=[f"ssqA{p3}"])
                S.op("act", lambda e: e.activation(out=junkA[:], in_=x_t, func=AF.Square, accum_out=ssq),
                     r=[f"x{xs}"], w=["junkA", f"ssqA{p3}"])
                rstd_ops(S, ssq, rs, D, [f"ssqA{p3}"], [f"rsA{p3}"])
                S.op("dve", lambda e: e.scalar_tensor_tensor(out=tmpA[:], in0=x_t, scalar=rs, in1=mod[:, GS1],
                                                             op0=ALU.mult, op1=ALU.mult),
                     r=[f"x{xs}", f"rsA{p3}", "mod1"], w=["tmpA"])
                S.op("dve", lambda e: e.tensor_tensor(out=hbf[:, 0, :], in0=tmpA[:], in1=mod[:, SH1], op=ALU.add),
                     r=["tmpA", "mod0"], w=["hbf"])
                if cut == -2:
                    if j == 0:
                        dump(S, 0, tmpA[:, :], ["hbf", "tmpA"], 1024)
                    return
                tb = bank_bf(0)
                for kc in range(8):
                    S.op("pe", lambda e, kc=kc: e.transpose(out=tb[:, kc * 128:(kc + 1) * 128],
                                                            in_=hbf[:, 0, kc * 128:(kc + 1) * 128], identity=idb[:]),
                         r=["hbf", "idb"], w=["bk0"])
                S.op("act", lambda e: e.copy(out=hT[:, pa, :, :], in_=tb[:, 0:1024].rearrange("p (k t) -> p k t", k=8)),
                     r=["bk0"], w=[f"hT{pa}"])
                if cut == -3:
                    if j == 0:
                        dump(S, 0, tmpA[:, :], [f"hT{pa}", "tmpA"], 1024)
                    return
                for kc in range(8):
                    S.op("pe", lambda e, kc=kc: e.matmul(banks[1][:, 0:128], lhsT=hT[:, pa, kc, :],
                                                         rhs=win[:, kc, 640:768], start=(kc == 0), stop=(kc == 7)),
                         r=[f"hT{pa}", "win"], w=["bk1"])
                for kc in range(8):
                    S.op("pe", lambda e, kc=kc: e.matmul(banks[1][:, 128:256], lhsT=win[:, kc, 512:640],
                                                         rhs=hT[:, pa, kc, :], start=(kc == 0), stop=(kc == 7)),
                         r=[f"hT{pa}", "win"], w=["bk1"])
                S.op("dve", lambda e: e.tensor_copy(out=vv[:, j, :], in_=banks[1][:, 0:128]), r=["bk1"], w=[f"v{j}"])
                S.op("act", lambda e: e.copy(out=kT[:, j, :], in_=banks[1][:, 128:256]), r=["bk1"], w=[f"k{j}"])
                if cut == -4:
                    if j == 0:
                        dump(S, 0, tmpA[:, :], [f"v{j}", f"k{j}", "tmpA"], 1024)
                    return
                if halo:
                    return
                for m in range(4):
                    for kc in range(8):
                        S.op("pe", lambda e, kc=kc, m=m: e.matmul(
                            banks[2][:, m * 128:(m + 1) * 128], lhsT=win[:, kc, m * 128:(m + 1) * 128],
                            rhs=hT[:, pa, kc, :], start=(kc == 0), stop=(kc == 7)),
                            r=[f"hT{pa}", "win"], w=["bk2"])
                S.op("act", lambda e: e.copy(out=qT[:, p3, :, :],
                                             in_=banks[2][:, :].rearrange("p (m t) -> p m t", m=4)),
                     r=["bk2"], w=[f"qT{p3}"])
                for (bi, c0, dst, key) in ((3, 768, ug, "ug"), (3, 1280, vg, "vg")):
                    for kc in range(8):
                        S.op("pe", lambda e, kc=kc, bi=bi, c0=c0: e.matmul(
                            banks[bi][:, :], lhsT=hT[:, pa, kc, :], rhs=win[:, kc, c0:c0 + 512],
                            start=(kc == 0), stop=(kc == 7)),
                            r=[f"hT{pa}", "win"], w=[f"bk{bi}"])
                    S.op("act", lambda e, bi=bi, dst=dst: e.activation(out=dst[:, p3, :], in_=banks[bi][:, :],
                                                                       func=AF.Gelu_apprx_tanh),
                         r=[f"bk{bi}"], w=[f"{key}{p3}"])

            def stageB(j, S):
                pa = j % 2
                xs = j % 3
                p3 = j % 3
                ti = j - 1
                x_t = xb[:, xs, :]
                if cut == 1:
                    dump(S, 0, vg[:, p3, :], [f"vg{p3}"], 512)
                    return
                for h in range(8):
                    half, m = h // 4, h % 4
                    sb_ = 4 + (h % 2)
                    pr0 = half * 64
                    S.op("pe", lambda e, m=m, pr0=pr0, sb_=sb_: e.matmul(
                        banks[sb_][:, 0:384], lhsT=qT[pr0:pr0 + 64, p3, m, :],
                        rhs=kT[pr0:pr0 + 64, j - 1:j + 2, :].rearrange("p a t -> p (a t)"), start=True, stop=True),
                        r=[f"qT{p3}", f"k{j-1}", f"k{j}", f"k{j+1}"], w=[f"bk{sb_}"])
                    S.op("dve", lambda e, h=h, sb_=sb_: e.scalar_tensor_tensor(
                        out=sall[:, h, :], in0=banks[sb_][:, 0:384], scalar=0.125, in1=bt[:, h, :],
                        op0=ALU.mult, op1=ALU.add),
                        r=[f"bk{sb_}", "bt"], w=[f"sall{h}"])
                sk = [f"sall{h}" for h in range(8)]
                if j == 1:
                    S.op("dve", lambda e: e.tensor_scalar(out=sall[:, :, 0:128], in0=sall[:, :, 0:128],
                                                          scalar1=pr[:, PROW["edge"]:PROW["edge"] + 1], scalar2=None,
                                                          op0=ALU.add), r=sk + ["pr"], w=sk)
                if j == NT:
                    S.op("dve", lambda e: e.tensor_scalar(out=sall[:, :, 256:384], in0=sall[:, :, 256:384],
                                                          scalar1=pr[:, PROW["edge"] + 1:PROW["edge"] + 2], scalar2=None,
                                                          op0=ALU.add), r=sk + ["pr"], w=sk)
                mx = st[:, pa, 8:16]
                nmx = st[:, pa, 16:24]
                rsum = st[:, pa, 24:32]
                S.op("dve", lambda e: e.tensor_reduce(out=mx, in_=sall[:, :, :], axis=AX.X, op=ALU.max),
                     r=sk, w=[f"mx{pa}"])
                sinkb = pr[:, PROW["sink"]:PROW["sink"] + 8]
                S.op("dve", lambda e: e.tensor_tensor(out=mx, in0=mx, in1=sinkb, op=ALU.max),
                     r=[f"mx{pa}", "pr"], w=[f"mx{pa}"])
                S.op("dve", lambda e: e.tensor_scalar(out=nmx, in0=mx, scalar1=-1.0, scalar2=None, op0=ALU.mult),
                     r=[f"mx{pa}"], w=[f"nmx{pa}"])
                S.op("pool", lambda e: e.memset(rsum, 0.0), w=[f"rsum{pa}"])
                for h in range(8):
                    S.op("act", lambda e, h=h: e.activation(out=pbf[:, h, :], in_=sall[:, h, :], func=AF.Exp,
                                                            bias=nmx[:, h:h + 1], scale=1.0,
                                                            accum_out=rsum[:, h:h + 1]),
                         r=[f"sall{h}", f"nmx{pa}"], w=[f"pbf{h}", f"rsum{pa}"])
                S.op("dve", lambda e: e.tensor_tensor(out=sm[:], in0=sinkb, in1=nmx, op=ALU.add),
                     r=["pr", f"nmx{pa}"], w=["sm"])
                S.op("act", lambda e: e.activation(out=sm[:], in_=sm[:], func=AF.Exp), r=["sm"], w=["sm"])
                S.op("dve", lambda e: e.tensor_tensor(out=sm[:], in0=sm[:], in1=rsum, op=ALU.add),
                     r=["sm", f"rsum{pa}"], w=["sm"])
                S.op("dve", lambda e: e.reciprocal(out=sm[:], in_=sm[:]), r=["sm"], w=["sm"])
                for hp in range(4):
                    bi = 6 + (hp % 2)
                    tb = bank_bf(bi)
                    for hh in range(2):
                        h = hp * 2 + hh
                        for a in range(3):
                            S.op("pe", lambda e, h=h, a=a, hh=hh, tb=tb: e.transpose(
                                out=tb[:, hh * 384 + a * 128: hh * 384 + (a + 1) * 128],
                                in_=pbf[:, h, a * 128:(a + 1) * 128], identity=idb[:]),
                                r=[f"pbf{h}", "idb"], w=[f"bk{bi}"])
                    eng = "act" if hp % 2 == 0 else "dve"
                    if eng == "act":
                        S.op("act", lambda e, hp=hp, tb=tb: e.copy(
                            out=pT[:, hp * 2:hp * 2 + 2, :], in_=tb[:, 0:768].rearrange("p (h c) -> p h c", h=2)),
                            r=[f"bk{bi}"], w=[f"pT{hp}"])
                    else:
                        S.op("dve", lambda e, hp=hp, tb=tb: e.tensor_copy(
                            out=pT[:, hp * 2:hp * 2 + 2, :], in_=tb[:, 0:768].rearrange("p (h c) -> p h c", h=2)),
                            r=[f"bk{bi}"], w=[f"pT{hp}"])
                for h in range(8):
                    kvh = h // 4
                    for a in range(3):
                        S.op("pe", lambda e, h=h, a=a, kvh=kvh: e.matmul(
                            banks[4][:, h * 64:(h + 1) * 64], lhsT=pT[:, h, a * 128:(a + 1) * 128],
                            rhs=vv[:, j - 1 + a, kvh * 64:(kvh + 1) * 64], start=(a == 0), stop=(a == 2)),
                            r=[f"pT{h // 2}", f"v{j - 1 + a}"], w=["bk4"])
                S.op("dve", lambda e: e.tensor_tensor(
                    out=attn[:, :].rearrange("p (h d) -> p h d", h=8),
                    in0=banks[4][:, :].rearrange("p (h d) -> p h d", h=8),
                    in1=bcast(sm[:, :].unsqueeze(2), [128, 8, 64]), op=ALU.mult),
                    r=["bk4", "sm"], w=["attn"])
                if cut == 2:
                    dump(S, 0, attn[:, :], ["attn"], 512)
                    return
                sa = st[:, pa, 2:3]
                ra = st[:, pa, 3:4]
                S.op("pool", lambda e: e.memset(sa, 0.0), w=[f"sa{pa}"])
                S.op("act", lambda e: e.activation(out=junkB[:, 0:512], in_=attn[:], func=AF.Square, accum_out=sa),
                     r=["attn"], w=["junkB", f"sa{pa}"])
                rstd_ops(S, sa, ra, 512, [f"sa{pa}"], [f"ra{pa}"])
                S.op("dve", lambda e: e.scalar_tensor_tensor(
                    out=mix[:, 0:512], in0=attn[:], scalar=ra, in1=pr[:, PROW["g_attn"]:PROW["g_attn"] + 512],
                    op0=ALU.mult, op1=ALU.mult), r=["attn", f"ra{pa}", "pr"], w=["mixa"])
                s1_ = st[:, pa, 4:5]
                s2_ = st[:, pa, 5:6]
                mu = st[:, pa, 6:7]
                S.op("dve", lambda e: e.tensor_reduce(out=s1_, in_=vg[:, p3, :], axis=AX.X, op=ALU.add),
                     r=[f"vg{p3}"], w=[f"s1{pa}"])
                S.op("dve", lambda e: e.tensor_scalar(out=mu, in0=s1_, scalar1=-1.0 / 512, scalar2=None, op0=ALU.mult),
                     r=[f"s1{pa}"], w=[f"mu{pa}"])
                S.op("dve", lambda e: e.tensor_scalar(out=gmt[:], in0=vg[:, p3, :], scalar1=mu, scalar2=None,
                                                      op0=ALU.add), r=[f"vg{p3}", f"mu{pa}"], w=["gmt"])
                S.op("pool", lambda e: e.memset(s2_, 0.0), w=[f"s2{pa}"])
                S.op("act", lambda e: e.activation(out=junkB[:, 0:512], in_=gmt[:], func=AF.Square, accum_out=s2_),
                     r=["gmt"], w=["junkB", f"s2{pa}"])
                rstd_ops(S, s2_, s2_, 512, [f"s2{pa}"], [f"s2{pa}"])
                S.op("dve", lambda e: e.scalar_tensor_tensor(
                    out=gmt[:], in0=gmt[:], scalar=s2_, in1=pr[:, PROW["ln_g"]:PROW["ln_g"] + 512],
                    op0=ALU.mult, op1=ALU.mult), r=["gmt", f"s2{pa}", "pr"], w=["gmt"])
                S.op("dve", lambda e: e.tensor_tensor(out=vn[:], in0=gmt[:], in1=pr[:, PROW["ln_b"]:PROW["ln_b"] + 512],
                                                      op=ALU.add), r=["gmt", "pr"], w=["vn"])
                for g in range(8):
                    S.op("pe", lambda e, g=g: e.matmul(banks[5][:, g * 64:(g + 1) * 64], lhsT=wsb[:, g, :],
                                                       rhs=vn[:, g * 64:(g + 1) * 64], start=True, stop=True),
                         r=["wsb", "vn"], w=["bk5"])
                S.op("dve", lambda e: e.tensor_tensor(
                    out=gmt[:, :].rearrange("p (g c) -> p g c", g=8),
                    in0=banks[5][:, :].rearrange("p (g c) -> p g c", g=8),
                    in1=bcast(bsb[:, :].unsqueeze(2), [128, 8, 64]), op=ALU.add),
                    r=["bk5", "bsb"], w=["gmt"])
                S.op("dve", lambda e: e.tensor_tensor(out=gmt[:], in0=gmt[:], in1=ug[:, p3, :], op=ALU.mult),
                     r=["gmt", f"ug{p3}"], w=["gmt"])
                S.op("pool", lambda e: e.memset(s1_, 0.0), w=[f"s1{pa}"])
                S.op("act", lambda e: e.activation(out=junkB[:, 0:512], in_=gmt[:], func=AF.Square, accum_out=s1_),
                     r=["gmt"], w=["junkB", f"s1{pa}"])
                rstd_ops(S, s1_, s1_, 512, [f"s1{pa}"], [f"s1{pa}"])
                S.op("dve", lambda e: e.scalar_tensor_tensor(
                    out=mix[:, 512:1024], in0=gmt[:], scalar=s1_, in1=pr[:, PROW["g_gmlp"]:PROW["g_gmlp"] + 512],
                    op0=ALU.mult, op1=ALU.mult), r=["gmt", f"s1{pa}", "pr"], w=["mixg"])
                if cut == 3:
                    dump(S, 0, gmt[:, :], ["gmt"], 512)
                    return
                tb = bank_bf(6)
                for kc in range(8):
                    S.op("pe", lambda e, kc=kc: e.transpose(out=tb[:, kc * 128:(kc + 1) * 128],
                                                            in_=mix[:, kc * 128:(kc + 1) * 128], identity=idb[:]),
                         r=["mixa", "mixg", "idb"], w=["bk6"])
                S.op("act", lambda e: e.copy(out=mixT[:, :, :], in_=tb[:, 0:1024].rearrange("p (k t) -> p k t", k=8)),
                     r=["bk6"], w=["mixT"])
                so = st[:, pa, 2:3]
                so2 = st[:, pa, 3:4]
                S.op("pool", lambda e: e.memset(st[:, pa, 2:4], 0.0), w=[f"sa{pa}", f"ra{pa}"])
                for hf in range(2):
                    for kc in range(8):
                        S.op("pe", lambda e, kc=kc, hf=hf: e.matmul(
                            banks[4 + hf][:, :], lhsT=mixT[:, kc, :], rhs=wout[:, kc, hf * 512:(hf + 1) * 512],
                            start=(kc == 0), stop=(kc == 7)), r=["mixT", "wout"], w=[f"bk{4 + hf}"])
                    S.op("act", lambda e, hf=hf: e.activation(
                        out=junkB[:, 0:512], in_=banks[4 + hf][:, :], func=AF.Square,
                        accum_out=st[:, pa, 2 + hf:3 + hf]),
                        r=[f"bk{4 + hf}"], w=["junkB", f"sa{pa}" if hf == 0 else f"ra{pa}"])
                S.op("dve", lambda e: e.tensor_tensor(out=so, in0=so, in1=so2, op=ALU.add),
                     r=[f"sa{pa}", f"ra{pa}"], w=[f"sa{pa}"])
                rstd_ops(S, so, so, D, [f"sa{pa}"], [f"sa{pa}"])
                for hf in range(2):
                    sl = slice(hf * 512, (hf + 1) * 512)
                    S.op("dve", lambda e, hf=hf, sl=sl: e.scalar_tensor_tensor(
                        out=tmpB[:, sl], in0=banks[4 + hf][:, :], scalar=so, in1=mod[:, D * 2 + hf * 512:D * 2 + (hf + 1) * 512],
                        op0=ALU.mult, op1=ALU.mult), r=[f"bk{4 + hf}", f"sa{pa}", "mod2"], w=["tmpB"])
                S.op("dve", lambda e: e.tensor_tensor(out=x_t, in0=x_t, in1=tmpB[:], op=ALU.add),
                     r=[f"x{xs}", "tmpB"], w=[f"x{xs}"])
                if stage == 1:
                    S.dma("sp", "st1", lambda e: e.dma_start(out=out[ti, :, :], in_=x_t), r=[f"x{xs}"], w=["outd"])
                else:
                    S.dma("sp", "st1", lambda e: e.dma_start(out=x1d[ti, :, :], in_=x_t), r=[f"x{xs}"], w=["x1d"])
                s3 = st[:, pa, 4:5]
                S.op("pool", lambda e: e.memset(s3, 0.0), w=[f"s1{pa}"])
                S.op("act", lambda e: e.activation(out=junkB[:], in_=x_t, func=AF.Square, accum_out=s3),
                     r=[f"x{xs}"], w=["junkB", f"s1{pa}"])
                rstd_ops(S, s3, s3, D, [f"s1{pa}"], [f"s1{pa}"])
                S.op("dve", lambda e: e.scalar_tensor_tensor(out=h2f[:], in0=x_t, scalar=s3, in1=mod[:, GS2],
                                                             op0=ALU.mult, op1=ALU.mult),
                     r=[f"x{xs}", f"s1{pa}", "mod4"], w=["h2f"])
                S.op("dve", lambda e: e.tensor_tensor(out=h2f[:], in0=h2f[:], in1=mod[:, SH2], op=ALU.add),
                     r=["h2f", "mod3"], w=["h2f"])
                if stage >= 4:
                    S.op("act", lambda e: e.copy(out=mix[:], in_=h2f[:]), r=["h2f"], w=["mixa", "mixg"])
                    S.dma("sp", "h2st", lambda e: e.dma_start(out=h2loc[ti * 128:(ti + 1) * 128, :], in_=mix[:]),
                          r=["mixa", "mixg"], w=["h2loc"])
                for hf in range(2):
                    for kk in range(4):
                        kc = hf * 4 + kk
                        S.op("pe", lambda e, kc=kc, kk=kk, hf=hf: e.transpose(
                            out=banks[6 + hf][:, kk * 128:(kk + 1) * 128], in_=h2f[:, kc * 128:(kc + 1) * 128],
                            identity=idf[:]), r=["h2f", "idf"], w=[f"bk{6 + hf}"])
                    S.op("act", lambda e, hf=hf: e.copy(
                        out=h2Tf[:, hf * 4:(hf + 1) * 4, :], in_=banks[6 + hf][:, :].rearrange("p (k t) -> p k t", k=4)),
                        r=[f"bk{6 + hf}"], w=[f"h2Tf{hf}"])
                    if stage == 3:
                        S.dma("sp", "h2st", lambda e, hf=hf: e.dma_start(
                            out=h2Td[:, hf * 4:(hf + 1) * 4, ti * 128:(ti + 1) * 128], in_=h2Tf[:, hf * 4:(hf + 1) * 4, :]),
                            r=[f"h2Tf{hf}"], w=["h2Td"])
                for kc in range(8):
                    S.op("pe", lambda e, kc=kc: e.matmul(banks[4][:, 0:NE], lhsT=h2Tf[:, kc, :], rhs=wrs[:, kc, :],
                                                         start=(kc == 0), stop=(kc == 7)),
                         r=["h2Tf0", "h2Tf1", "wrs"], w=["bk4"])
                lm = st[:, pa, 6:7]
                ls = st[:, pa, 7:8]
                S.op("dve", lambda e: e.tensor_reduce(out=lm, in_=banks[4][:, 0:NE], axis=AX.X, op=ALU.max),
                     r=["bk4"], w=[f"mu{pa}"])
                S.op("dve", lambda e: e.tensor_scalar(out=lm, in0=lm, scalar1=-1.0, scalar2=None, op0=ALU.mult),
                     r=[f"mu{pa}"], w=[f"mu{pa}"])
                S.op("pool", lambda e: e.memset(ls, 0.0), w=[f"ls{pa}"])
                S.op("act", lambda e: e.activation(out=affo[:, ti, :], in_=banks[4][:, 0:NE], func=AF.Exp,
                                                   bias=lm, scale=1.0, accum_out=ls),
                     r=["bk4", f"mu{pa}"], w=["affo", f"ls{pa}"])
                S.op("dve", lambda e: e.reciprocal(out=ls, in_=ls), r=[f"ls{pa}"], w=[f"ls{pa}"])
                S.op("dve", lambda e: e.tensor_scalar(out=affo[:, ti, :], in0=affo[:, ti, :], scalar1=ls, scalar2=None,
                                                      op0=ALU.mult), r=["affo", f"ls{pa}"], w=["affo"])
                S.op("pe", lambda e: e.transpose(out=banks[4][0:NE, 128:256], in_=affo[:, ti, :], identity=idf[:]),
                     r=["affo", "idf"], w=["bk4"])
                S.op("act", lambda e: e.copy(out=affT[:, ti * 128:(ti + 1) * 128], in_=banks[4][0:NE, 128:256]),
                     r=["bk4"], w=["affT"])

            class Rec:
                def __init__(self):
                    self.calls = []

                def op(self, *a, **k):
                    self.calls.append(("op", a, k))

                def dma(self, *a, **k):
                    self.calls.append(("dma", a, k))

            def replay(calls):
                for kind, a, k in calls:
                    getattr(S, kind)(*a, **k)

            def merge(ca, cb):
                outl = []
                na, nb = len(ca), len(cb)
                ia_ = ib_ = 0
                while ia_ < na or ib_ < nb:
                    if ib_ >= nb or (ia_ < na and ia_ * nb <= ib_ * na):
                        outl.append(ca[ia_]); ia_ += 1
                    else:
                        outl.append(cb[ib_]); ib_ += 1
                return outl

            for j0 in range(3):
                r0 = Rec(); stageA(j0, r0); replay(r0.calls)
            for j in range(1, (NT if cut >= 90 else 1) + 1):
                ra, rb = Rec(), Rec()
                if j + 2 <= NT + 1:
                    stageA(j + 2, ra)
                stageB(j, rb)
                replay(merge(ra.calls, rb.calls))
            fw = []
            if stage == 1:
                fw = [("c_st1", S.chan["st1"] * 16)]
            else:
                S.dma("sp", "ag", lambda e: e.dma_start(out=agi[:, :], in_=affT[:, :]), r=["affT"], w=["agi"])
                fw = [("c_st1", S.chan["st1"] * 16), ("c_ag", 16), ("c_h2st", S.chan["h2st"] * 16)]
            sems = {n: gs.enter_context(nc.semaphore(f"b1_{n}")) for n in S.sem_names()}
            S.emit(sems, final_waits=fw)
        if stage == 1:
            return nc

        def moe_expert_sharded():
            with ExitStack() as s3:
                S = Sched(nc, "b3")
                xs = SB("xs", [128, 2, D], BF16, s3)
                xsT = SB("xsT", [128, 8, CAP], BF16, s3)
                hid = SB("hid", [128, NFC, CAP], BF16, s3)
                wdb = SB("wdb", [128, NFC, D], BF16, s3)
                wgs = SB("wgs", [128, 2, 8, 128], F32, s3)
                wus = SB("wus", [128, 2, 8, 128], F32, s3)
                wgb = SB("wgb", [128, 2, 8, 128], BF16, s3)
                wub = SB("wub", [128, 2, 8, 128], BF16, s3)
                wds = SB("wds", [128, 2, D], F32, s3)
                sg = SB("sg", [128, 2, 512], F32, s3)
                orow = SB("orow", [128, 2, D], BF16, s3)
                def coll_pair(lpp):
                    S.coll("ago", lambda e, lpp=lpp: e.collective_compute(
                        "AllGather", ALU.bypass, replica_groups=[list(range(8))],
                        ins=[outloc[lpp * CAP:(lpp + 1) * CAP, :]], outs=[outall[lpp * 8 * CAP:(lpp + 1) * 8 * CAP, :]]),
                        r=[f"outloc{lpp}"], w=["outall"])
                def gather_T(lp):
                    for sc in range(8):
                        xb_ = sc % 2
                        col = lp * 8 + sc
                        S.dma("pool", f"g{xb_}", lambda e, xb_=xb_, col=col: e.indirect_dma_start(
                            out=xs[:, xb_, :], out_offset=None, in_=h2all[:, :],
                            in_offset=bass.IndirectOffsetOnAxis(ap=lidx[:, col:col + 1], axis=0)),
                            r=["lidx"], w=[f"xs{xb_}"])
                        tb = bank_bf(xb_)
                        for kc in range(8):
                            S.op("pe", lambda e, kc=kc, xb_=xb_, tb=tb: e.transpose(
                                out=tb[:, kc * 128:(kc + 1) * 128], in_=xs[:, xb_, kc * 128:(kc + 1) * 128], identity=idb[:]),
                                r=[f"xs{xb_}", "idb"], w=[f"bk{xb_}"])
                        if sc % 2 == 0:
                            S.op("act", lambda e, sc=sc, tb=tb: e.copy(
                                out=xsT[:, :, sc * 128:(sc + 1) * 128], in_=tb[:, 0:1024].rearrange("p (k t) -> p k t", k=8)),
                                r=[f"bk{xb_}"], w=["xsT"])
                        else:
                            S.op("dve", lambda e, sc=sc, tb=tb: e.tensor_copy(
                                out=xsT[:, :, sc * 128:(sc + 1) * 128], in_=tb[:, 0:1024].rearrange("p (k t) -> p k t", k=8)),
                                r=[f"bk{xb_}"], w=["xsT"])

                def loads(lp, fc):
                    el, pa = lp // 2, fc % 2
                    S.dma("sp", f"wg{pa}", lambda e: e.dma_start(out=wgs[:, pa], in_=wg[el, fc]), w=[f"wgs{pa}"])
                    S.dma("sp", f"wu{pa}", lambda e: e.dma_start(out=wus[:, pa], in_=wu[el, fc]), w=[f"wus{pa}"])

                def casts(lp, fc):
                    pa = fc % 2
                    S.op("act", lambda e: e.copy(out=wgb[:, pa], in_=wgs[:, pa]), r=[f"wgs{pa}"], w=[f"wgb{pa}"])
                    S.op("dve", lambda e: e.tensor_copy(out=wub[:, pa], in_=wus[:, pa]), r=[f"wus{pa}"], w=[f"wub{pa}"])

                def pre(lp):
                    loads(lp, 0)
                    loads(lp, 1)
                    casts(lp, 0)

                def stage1(lp):
                    el = lp // 2
                    for fc in range(NFC):
                        pa = fc % 2
                        if fc + 2 < NFC:
                            loads(lp, fc + 2)
                        if lp % 2 == 0:
                            S.dma("sp", f"wd{pa}", lambda e, fc=fc, pa=pa: e.dma_start(out=wds[:, pa, :], in_=wd[el, fc]),
                                  w=[f"wds{pa}"])
                        if fc + 1 < NFC:
                            casts(lp, fc + 1)
                        if lp % 2 == 0:
                            if pa == 0:
                                S.op("act", lambda e, fc=fc, pa=pa: e.copy(out=wdb[:, fc, :], in_=wds[:, pa, :]),
                                     r=[f"wds{pa}"], w=[f"wdb{fc}"])
                            else:
                                S.op("dve", lambda e, fc=fc, pa=pa: e.tensor_copy(out=wdb[:, fc, :], in_=wds[:, pa, :]),
                                     r=[f"wds{pa}"], w=[f"wdb{fc}"])
                        for nh in range(2):
                            ga, ub = 2 + 2 * nh, 3 + 2 * nh
                            for kc in range(8):
                                S.op("pe", lambda e, kc=kc, pa=pa, nh=nh, ga=ga: e.matmul(
                                    banks[ga][:, :], lhsT=wgb[:, pa, kc, :], rhs=xsT[:, kc, nh * 512:(nh + 1) * 512],
                                    start=(kc == 0), stop=(kc == 7)), r=[f"wgb{pa}", "xsT"], w=[f"bk{ga}"])
                            for kc in range(8):
                                S.op("pe", lambda e, kc=kc, pa=pa, nh=nh, ub=ub: e.matmul(
                                    banks[ub][:, :], lhsT=wub[:, pa, kc, :], rhs=xsT[:, kc, nh * 512:(nh + 1) * 512],
                                    start=(kc == 0), stop=(kc == 7)), r=[f"wub{pa}", "xsT"], w=[f"bk{ub}"])
                            S.op("act", lambda e, nh=nh, ga=ga: e.activation(out=sg[:, nh, :], in_=banks[ga][:, :], func=AF.Silu),
                                 r=[f"bk{ga}"], w=[f"sg{nh}"])
                            S.op("dve", lambda e, nh=nh, ub=ub, fc=fc: e.tensor_tensor(
                                out=hid[:, fc, nh * 512:(nh + 1) * 512], in0=sg[:, nh, :], in1=banks[ub][:, :], op=ALU.mult),
                                r=[f"sg{nh}", f"bk{ub}"], w=["hid"])

                def stage2(lp):
                    for tt in range(8):
                        ob_ = tt % 2
                        for dh in range(2):
                            obk = 6 + dh
                            for fc in range(NFC):
                                S.op("pe", lambda e, fc=fc, tt=tt, dh=dh, obk=obk: e.matmul(
                                    banks[obk][:, :], lhsT=hid[:, fc, tt * 128:(tt + 1) * 128],
                                    rhs=wdb[:, fc, dh * 512:(dh + 1) * 512], start=(fc == 0), stop=(fc == NFC - 1)),
                                    r=["hid", f"wdb{fc}"], w=[f"bk{obk}"])
                            if dh == 0:
                                S.op("act", lambda e, ob_=ob_, obk=obk: e.copy(out=orow[:, ob_, 0:512], in_=banks[obk][:, :]),
                                     r=[f"bk{obk}"], w=[f"orow{ob_}"])
                            else:
                                S.op("dve", lambda e, ob_=ob_, obk=obk: e.tensor_copy(out=orow[:, ob_, 512:1024], in_=banks[obk][:, :]),
                                     r=[f"bk{obk}"], w=[f"orow{ob_}"])
                        S.dma("sp", f"os{ob_}", lambda e, lp=lp, tt=tt, ob_=ob_: e.dma_start(
                            out=outloc[lp * CAP + tt * 128: lp * CAP + (tt + 1) * 128, :], in_=orow[:, ob_, :]),
                            r=[f"orow{ob_}"], w=[f"outloc{lp}"])

                gather_T(0)
                pre(0)
                for lp in range(4):
                    stage1(lp)
                    if lp < 3:
                        gather_T(lp + 1)
                        pre(lp + 1)
                    stage2(lp)
                    if lp < 3:
                        coll_pair(lp)
                coll_pair(3)
                sems = {n: gs.enter_context(nc.semaphore(f"b3_{n}")) for n in S.sem_names()}
                S.emit(sems, final_waits=[(f"c_os{i}", S.chan[f"os{i}"] * 16) for i in range(2)] + [("c_ago", 4)])
            with ExitStack() as s4:
                S = Sched(nc, "b4")
                grow = SB("grow", [128, 4, D], BF16, s4)
                yac = SB("yac", [128, 2, D], F32, s4)
                x1t = SB("x1t", [128, 2, D], F32, s4)
                jk = SB("jk", [128, D], BF16, s4)
                st3 = SB("st3", [128, 4], F32, s4)
                epsc3 = pr[:, PROW["eps"]:PROW["eps"] + 1]
                breg = {}

                def bchk(e):
                    if "r" not in breg:
                        breg["r"] = e.to_reg(8 * 4 * CAP - 1)
                    return breg["r"]
                S.op("dve", lambda e: e.memset(grow[:], 0.0), w=[f"grow{i}" for i in range(4)])
                for ti in range(NT):
                    pa = ti % 2
                    S.dma("sp", f"x1l{pa}", lambda e, ti=ti, pa=pa: e.dma_start(out=x1t[:, pa, :], in_=x1d[ti, :, :]),
                          w=[f"x1t{pa}"])
                    S.op("dve", lambda e, pa=pa: e.memset(yac[:, pa, :], 0.0), w=[f"yac{pa}"])
                    for ex in range(NE):
                        gb = ex % 4
                        col = ti * NE + ex
                        S.dma("pool", f"gg{gb}", lambda e, gb=gb, col=col: e.indirect_dma_start(
                            out=grow[:, gb, :], out_offset=None, in_=outall[:, :],
                            in_offset=bass.IndirectOffsetOnAxis(ap=cidx[:, col:col + 1], axis=0),
                            bounds_check=bchk(e), oob_is_err=False), r=["cidx"], w=[f"grow{gb}"])
                        S.op("dve", lambda e, gb=gb, pa=pa, ti=ti, ex=ex: e.scalar_tensor_tensor(
                            out=yac[:, pa, :], in0=grow[:, gb, :], scalar=gm_all[:, ti, ex:ex + 1], in1=yac[:, pa, :],
                            op0=ALU.mult, op1=ALU.add), r=[f"grow{gb}", "gm_all", f"yac{pa}"], w=[f"yac{pa}"])
                    sq = st3[:, pa:pa + 1]
                    S.op("pool", lambda e, sq=sq: e.memset(sq, 0.0), w=[f"sq{pa}"])
                    S.op("act", lambda e, pa=pa, sq=sq: e.activation(out=jk[:], in_=yac[:, pa, :], func=AF.Square, accum_out=sq),
                         r=[f"yac{pa}"], w=["jk", f"sq{pa}"])
                    S.op("act", lambda e, sq=sq: e.activation(out=sq, in_=sq, func=AF.Ln, scale=1.0 / D, bias=epsc3),
                         r=[f"sq{pa}"], w=[f"sq{pa}"])
                    S.op("act", lambda e, sq=sq: e.activation(out=sq, in_=sq, func=AF.Exp, scale=-0.5),
                         r=[f"sq{pa}"], w=[f"sq{pa}"])
                    S.op("dve", lambda e, pa=pa, sq=sq: e.scalar_tensor_tensor(
                        out=yac[:, pa, :], in0=yac[:, pa, :], scalar=sq, in1=mod[:, GG2], op0=ALU.mult, op1=ALU.mult),
                        r=[f"yac{pa}", f"sq{pa}"], w=[f"yac{pa}"])
                    S.op("dve", lambda e, pa=pa: e.tensor_tensor(out=x1t[:, pa, :], in0=x1t[:, pa, :], in1=yac[:, pa, :],
                                                                 op=ALU.add), r=[f"yac{pa}", f"x1t{pa}"], w=[f"x1t{pa}"])
                    S.dma("sp", "ost", lambda e, ti=ti, pa=pa: e.dma_start(out=out[ti, :, :], in_=x1t[:, pa, :]),
                          r=[f"x1t{pa}"], w=["outd"])
                sems = {n: gs.enter_context(nc.semaphore(f"b4_{n}")) for n in S.sem_names()}
                S.emit(sems, final_waits=[("c_ost", S.chan["ost"] * 16)])

        def routing_tail(S, s2, affA, msk, lo, selo_s, thr, mk2, lobc):
            ones_t = SB("ones_t", [128, NT * 128], F32, s2)
            cum = SB("cum", [128, NT * 128], F32, s2)
            selq_s = SB("selq_s", [128, 128], F32, s2)
            selp_s = SB("selp_s", [128, NE], F32, s2)
            iota_s = SB("iota_s", [128, CAP], F32, s2)
            tokf = SB("tokf", [128, 64, 2], F32, s2)
            tokb = SB("tokb", [128, 64, 2], BF16, s2)
            w2_s = SB("w2_s", [2, 1], F32, s2)
            cb_s = SB("cb_s", [128, NE], F32, s2)
            pb_s = SB("pb_s", [128, 32], F32, s2)
            offc = SB("offc", [128, 1], F32, s2)
            GTp = SB("GTp", [128, NT * NE], F32, s2)
            PTo = SB("PTo", [128, NT * NE], F32, s2)
            tq = SB("tq", [128, NT * NE], F32, s2)
            oh = SB("oh", [128, 2, CAP], BF16, s2)
            Ls = SB("Ls", [2, CAP], F32, s2)
            lf = SB("lf", [128, 32], F32, s2)
            for (dst, src, k) in ((selq_s, selq, "selq"), (selp_s, selpair, "selp"), (iota_s, iota_d, "iota"),
                                  (w2_s, w2_d, "w2"), (cb_s, cbase_d, "cb"), (pb_s, pbase_d, "pb")):
                S.dma("sp", "l4", group=True, fn=lambda e, dst=dst, src=src: e.dma_start(out=dst[:], in_=src[:, :]), w=[k])
            S.dma("sp", "l4", group=True, fn=lambda e: e.dma_start(out=tokf[:], in_=tokid_d[:, :, :]), w=["tokf"])
            S.op("act", lambda e: e.copy(out=tokb[:], in_=tokf[:]), r=["tokf"], w=["tokb"])
            S.op("pool", lambda e: e.memset(ones_t[:], 1.0), w=["ones_t"])
            S.op("dve", lambda e: e.tensor_copy(out=lobc[:], in_=bcast(lo, [128, 128])), r=["bs"], w=["lobc"])
            S.op("pe", lambda e: e.matmul(banks[1][:, 0:NE], lhsT=lobc[:], rhs=selo_s[:], start=True, stop=True),
                 r=["lobc", "selo"], w=["bk1"])
            S.op("dve", lambda e: e.tensor_copy(out=thr[:], in_=banks[1][:, 0:NE]), r=["bk1"], w=["thr"])
            S.op("dve", lambda e: e.tensor_tensor(out=mk2[:], in0=affo[:], in1=bcast(thr[:, :].unsqueeze(1), [128, NT, NE]),
                                                  op=ALU.is_ge), r=["thr", "affo"], w=["mk2"])
            S.op("dve", lambda e: e.tensor_tensor(out=gm_all[:], in0=mk2[:], in1=affo[:], op=ALU.mult),
                 r=["mk2", "affo"], w=["gm_all"])
            S.op("dve", lambda e: e.tensor_scalar(out=msk[:], in0=affA[:], scalar1=lo, scalar2=None, op0=ALU.is_ge),
                 r=["affA", "bs"], w=["msk"])
            S.op("dve", lambda e: e.tensor_tensor_scan(out=cum[:], data0=ones_t[:], data1=msk[:], initial=0.0,
                                                       op0=ALU.mult, op1=ALU.add), r=["ones_t", "msk"], w=["cum"])
            S.op("pe", lambda e: e.matmul(banks[0][:, 0:1], lhsT=selq_s[:], rhs=cum[:, NT * 128 - 1:NT * 128],
                                          start=True, stop=True), r=["selq", "cum"], w=["bk0"])
            S.op("dve", lambda e: e.tensor_copy(out=offc[:], in_=banks[0][:, 0:1]), r=["bk0"], w=["offc"])
            S.op("dve", lambda e: e.scalar_tensor_tensor(out=cum[:], in0=cum[:], scalar=offc[:, 0:1], in1=msk[:],
                                                         op0=ALU.add, op1=ALU.mult), r=["cum", "offc", "msk"], w=["cum"])
            S.op("dve", lambda e: e.tensor_scalar(out=cum[:], in0=cum[:], scalar1=-1.0, scalar2=None, op0=ALU.add),
                 r=["cum"], w=["cum"])
            for (bk_i, sel_t, selk, dst, dk) in ((2, selp_s, "selp", GTp, "GTp"), (3, selo_s, "selo", PTo, "PTo")):
                for ch in range(NT):
                    S.op("pe", lambda e, ch=ch, bk_i=bk_i, sel_t=sel_t: e.matmul(
                        banks[bk_i][:, ch * NE:(ch + 1) * NE], lhsT=cum[:, ch * 128:(ch + 1) * 128], rhs=sel_t[:],
                        start=True, stop=True), r=["cum", selk], w=[f"bk{bk_i}"])
                S.op("dve", lambda e, bk_i=bk_i, dst=dst: e.tensor_copy(out=dst[:], in_=banks[bk_i][:, 0:NT * NE]),
                     r=[f"bk{bk_i}"], w=[dk])
            S.op("dve", lambda e: e.tensor_scalar(out=tq[:], in0=PTo[:], scalar1=0.0, scalar2=None, op0=ALU.is_lt),
                 r=["PTo"], w=["tq"])
            S.op("dve", lambda e: e.scalar_tensor_tensor(out=tq[:], in0=tq[:], scalar=1.0e6, in1=PTo[:], op0=ALU.mult,
                                                         op1=ALU.add), r=["tq", "PTo"], w=["tq"])
            S.op("dve", lambda e: e.tensor_tensor(out=tq[:, :].rearrange("p (c n) -> p c n", n=NE),
                                                  in0=tq[:, :].rearrange("p (c n) -> p c n", n=NE),
                                                  in1=bcast(cb_s[:, :].unsqueeze(1), [128, NT, NE]), op=ALU.add),
                 r=["tq", "cb"], w=["tq"])
            S.op("dve", lambda e: e.tensor_copy(out=cidx[:], in_=tq[:]), r=["tq"], w=["cidx"])
            for lp in range(4):
                pb0 = 4 + 2 * (lp % 2)
                for q in range(4):
                    n = lp * 4 + q
                    for ch in range(NT):
                        tile_i = q * NT + ch
                        ob = tile_i % 2
                        S.op("dve", lambda e, ch=ch, n=n, ob=ob: e.tensor_scalar(
                            out=oh[:, ob, :], in0=iota_s[:], scalar1=GTp[:, ch * NE + n:ch * NE + n + 1], scalar2=None,
                            op0=ALU.is_equal), r=["iota", "GTp"], w=[f"oh{ob}"])
                        for hf in range(2):
                            S.op("pe", lambda e, tile_i=tile_i, ob=ob, hf=hf, pb0=pb0: e.matmul(
                                banks[pb0 + hf][0:2, :], lhsT=tokb[:, tile_i, :], rhs=oh[:, ob, hf * 512:(hf + 1) * 512],
                                start=(tile_i == 0), stop=(tile_i == 63)), r=["tokb", f"oh{ob}"], w=[f"bk{pb0 + hf}"])
                for hf in range(2):
                    S.op("act", lambda e, hf=hf, pb0=pb0: e.copy(out=Ls[0:2, hf * 512:(hf + 1) * 512], in_=banks[pb0 + hf][0:2, :]),
                         r=[f"bk{pb0 + hf}"], w=["Ls"])
                for sc in range(8):
                    S.op("pe", lambda e, sc=sc, lp=lp: e.matmul(
                        banks[1][:, lp * 8 + sc:lp * 8 + sc + 1], lhsT=Ls[0:2, sc * 128:(sc + 1) * 128], rhs=w2_s[0:2, 0:1],
                        start=True, stop=True), r=["Ls", "w2"], w=["bk1"])
            S.op("dve", lambda e: e.tensor_tensor(out=lf[:], in0=banks[1][:, 0:32], in1=pb_s[:], op=ALU.add),
                 r=["bk1", "pb"], w=["lf"])
            S.op("dve", lambda e: e.tensor_copy(out=lidx[:], in_=lf[:]), r=["lf"], w=["lidx"])

        with ExitStack() as s2:
            S = Sched(nc, "b2")
            affA = SB("affA", [128, NT * 128], F32, s2)
            msk = SB("msk", [128, NT * 128], F32, s2)
            selm_s = SB("selm_s", [128, 128], F32, s2)
            selo_s = SB("selo_s", [128, NE], F32, s2)
            bs = SB("bs", [128, 8], F32, s2)
            lobc = SB("lobc", [128, 128], F32, s2)
            thr = SB("thr", [128, NE], F32, s2)
            mk2 = SB("mk2", [128, NT, NE], F32, s2)
            S.coll("agc", lambda e: e.collective_compute("AllGather", ALU.bypass, replica_groups=[list(range(8))],
                                                         ins=[agi.ap().opt()], outs=[ago.ap().opt()]), w=["ago"])
            if stage >= 4:
                S.coll("agh", lambda e: e.collective_compute("AllGather", ALU.bypass, replica_groups=[list(range(8))],
                                                             ins=[h2loc.ap().opt()], outs=[h2all.ap().opt()]), w=["h2all"])
            S.dma("sp", "l2", group=True, fn=lambda e: e.dma_start(out=selm_s[:], in_=selm[:, :]), w=["selm"])
            S.dma("sp", "l2", group=True, fn=lambda e: e.dma_start(out=selo_s[:], in_=selown[:, :]), w=["selo"])
            S.dma("sp", "l3", lambda e: e.dma_start(out=affA[:], in_=ago[:, :]), r=["ago"], w=["affA"])
            lo, hi, mid, cnt, ge, t1 = [bs[:, i:i + 1] for i in range(6)]
            S.op("dve", lambda e: e.memset(bs[:], 0.0), w=["bs"])
            S.op("dve", lambda e: e.memset(hi, 1.0), r=["bs"], w=["bs"])
            S.op("dve", lambda e: e.memset(mid, 0.5), r=["bs"], w=["bs"])
            for it in range(28):
                S.op("dve", lambda e: e.tensor_scalar(out=msk[:], in0=affA[:], scalar1=mid, scalar2=None, op0=ALU.is_ge),
                     r=["affA", "bs"], w=["msk"])
                S.op("dve", lambda e: e.tensor_reduce(out=cnt, in_=msk[:], axis=AX.X, op=ALU.add), r=["msk"], w=["bs"])
                S.op("pe", lambda e: e.matmul(banks[0][:, 0:1], lhsT=selm_s[:], rhs=cnt, start=True, stop=True),
                     r=["selm", "bs"], w=["bk0"])
                S.op("dve", lambda e: e.tensor_scalar(out=ge, in0=banks[0][:, 0:1], scalar1=float(CAP), scalar2=None,
                                                      op0=ALU.is_ge), r=["bk0"], w=["bs"])
                S.op("dve", lambda e: e.tensor_tensor(out=t1, in0=mid, in1=ge, op=ALU.mult), r=["bs"], w=["bs"])
                S.op("dve", lambda e: e.tensor_tensor(out=lo, in0=lo, in1=t1, op=ALU.max), r=["bs"], w=["bs"])
                S.op("dve", lambda e: e.scalar_tensor_tensor(out=t1, in0=ge, scalar=4.0, in1=mid, op0=ALU.mult, op1=ALU.add),
                     r=["bs"], w=["bs"])
                S.op("dve", lambda e: e.tensor_tensor(out=hi, in0=hi, in1=t1, op=ALU.min), r=["bs"], w=["bs"])
                S.op("dve", lambda e: e.tensor_scalar(out=t1, in0=hi, scalar1=0.5, scalar2=None, op0=ALU.mult),
                     r=["bs"], w=["bs"])
                S.op("dve", lambda e: e.scalar_tensor_tensor(out=mid, in0=lo, scalar=0.5, in1=t1, op0=ALU.mult, op1=ALU.add),
                     r=["bs"], w=["bs"])
            if stage == 3:
                S.op("dve", lambda e: e.tensor_copy(out=lobc[:], in_=bcast(lo, [128, 128])), r=["bs"], w=["lobc"])
                S.op("pe", lambda e: e.matmul(banks[1][:, 0:NE], lhsT=lobc[:], rhs=selo_s[:], start=True, stop=True),
                     r=["lobc", "selo"], w=["bk1"])
                S.op("dve", lambda e: e.tensor_copy(out=thr[:], in_=banks[1][:, 0:NE]), r=["bk1"], w=["thr"])
                S.op("dve", lambda e: e.tensor_tensor(out=mk2[:], in0=affo[:], in1=bcast(thr[:, :].unsqueeze(1), [128, NT, NE]),
                                                      op=ALU.is_ge), r=["thr", "affo"], w=["mk2"])
                S.op("dve", lambda e: e.tensor_tensor(out=gm_all[:], in0=mk2[:], in1=affo[:], op=ALU.mult),
                     r=["mk2", "affo"], w=["gm_all"])
            else:
                routing_tail(S, s2, affA, msk, lo, selo_s, thr, mk2, lobc)
            sems = {n: gs.enter_context(nc.semaphore(f"b2_{n}")) for n in S.sem_names()}
            S.emit(sems, final_waits=([("c_agh", 1)] if stage >= 4 else []))

        if stage >= 4:
            moe_expert_sharded()
            return nc
        with ExitStack() as s3:
            S = Sched(nc, "b3")
            HT = 512
            h2s = SB("h2s", [128, 8, 128], F32, s3)
            h2b = SB("h2b", [128, 8, HT], BF16, s3)
            hid = SB("hid", [128, NFC, HT], BF16, s3)
            wdb = SB("wdb", [128, NFC, D], BF16, s3)
            yac = SB("yac", [128, 4, D], F32, s3)
            wgs = SB("wgs", [128, 1, 8, 128], F32, s3)
            wus = SB("wus", [128, 1, 8, 128], F32, s3)
            wgb = SB("wgb", [128, 2, 8, 128], BF16, s3)
            wub = SB("wub", [128, 2, 8, 128], BF16, s3)
            wds = SB("wds", [128, 2, D], F32, s3)
            sg = SB("sg", [128, 2, 512], F32, s3)
            x1t = SB("x1t", [128, 2, D], F32, s3)
            jk = SB("jk", [128, D], BF16, s3)
            st3 = SB("st3", [128, 4], F32, s3)
            epsc3 = pr[:, PROW["eps"]:PROW["eps"] + 1]
            for half in range(4):
                for q4 in range(4):
                    S.dma("sp", "h2l", lambda e, half=half, q4=q4: e.dma_start(
                        out=h2s[:], in_=h2Td[:, :, half * HT + q4 * 128: half * HT + (q4 + 1) * 128]), w=["h2s"])
                    S.op("dve", lambda e, q4=q4: e.tensor_copy(out=h2b[:, :, q4 * 128:(q4 + 1) * 128], in_=h2s[:]),
                         r=["h2s"], w=["h2b"])
                S.op("dve", lambda e: e.memset(yac[:], 0.0), w=["yac"] + [f"yac{a}{b}" for a in range(4) for b in range(2)])
                for ex in range(NE):
                    for fc in range(NFC):
                        pa = fc % 2
                        S.dma("sp", "wg0", lambda e, ex=ex, fc=fc, pa=pa: e.dma_start(out=wgs[:, 0], in_=wg[ex, fc]),
                              w=["wgs"])
                        S.dma("sp", "wu0", lambda e, ex=ex, fc=fc, pa=pa: e.dma_start(out=wus[:, 0], in_=wu[ex, fc]),
                              w=["wus"])
                        S.op("act", lambda e, pa=pa: e.copy(out=wgb[:, pa], in_=wgs[:, 0]), r=["wgs"], w=[f"wgb{pa}"])
                        S.op("dve", lambda e, pa=pa: e.tensor_copy(out=wub[:, pa], in_=wus[:, 0]), r=["wus"], w=[f"wub{pa}"])
                        for nh in range(1):
                            ga, ub = 2 * (fc % 2), 2 * (fc % 2) + 1
                            for kc in range(8):
                                S.op("pe", lambda e, kc=kc, pa=pa, nh=nh, ga=ga: e.matmul(
                                    banks[ga][:, :], lhsT=wgb[:, pa, kc, :], rhs=h2b[:, kc, nh * 512:(nh + 1) * 512],
                                    start=(kc == 0), stop=(kc == 7)), r=[f"wgb{pa}", "h2b"], w=[f"bk{ga}"])
                            for kc in range(8):
                                S.op("pe", lambda e, kc=kc, pa=pa, nh=nh, ub=ub: e.matmul(
                                    banks[ub][:, :], lhsT=wub[:, pa, kc, :], rhs=h2b[:, kc, nh * 512:(nh + 1) * 512],
                                    start=(kc == 0), stop=(kc == 7)), r=[f"wub{pa}", "h2b"], w=[f"bk{ub}"])
                            S.op("act", lambda e, nh=nh, ga=ga: e.activation(out=sg[:, nh, :], in_=banks[ga][:, :], func=AF.Silu),
                                 r=[f"bk{ga}"], w=[f"sg{nh}"])
                            S.op("dve", lambda e, nh=nh, ub=ub, fc=fc: e.tensor_tensor(
                                out=hid[:, fc, nh * 512:(nh + 1) * 512], in0=sg[:, nh, :], in1=banks[ub][:, :], op=ALU.mult),
                                r=[f"sg{nh}", f"bk{ub}"], w=["hid"])
                    for fc in range(NFC):
                        pa = fc % 2
                        S.dma("sp", f"wd{pa}", lambda e, ex=ex, fc=fc, pa=pa: e.dma_start(out=wds[:, pa, :], in_=wd[ex, fc]),
                              w=[f"wds{pa}"])
                        if pa == 0:
                            S.op("act", lambda e, fc=fc, pa=pa: e.copy(out=wdb[:, fc, :], in_=wds[:, pa, :]),
                                 r=[f"wds{pa}"], w=["wdb"])
                        else:
                            S.op("dve", lambda e, fc=fc, pa=pa: e.tensor_copy(out=wdb[:, fc, :], in_=wds[:, pa, :]),
                                 r=[f"wds{pa}"], w=["wdb"])
                    for tt in range(4):
                        for dh in range(2):
                            ob = 4 + (tt * 2 + dh) % 4
                            for fc in range(NFC):
                                S.op("pe", lambda e, fc=fc, tt=tt, dh=dh, ob=ob: e.matmul(
                                    banks[ob][:, :], lhsT=hid[:, fc, tt * 128:(tt + 1) * 128],
                                    rhs=wdb[:, fc, dh * 512:(dh + 1) * 512], start=(fc == 0), stop=(fc == NFC - 1)),
                                    r=["hid", "wdb"], w=[f"bk{ob}"])
                            S.op("dve", lambda e, tt=tt, dh=dh, ob=ob, ex=ex, half=half: e.scalar_tensor_tensor(
                                out=yac[:, tt, dh * 512:(dh + 1) * 512], in0=banks[ob][:, :],
                                scalar=gm_all[:, half * 4 + tt, ex:ex + 1], in1=yac[:, tt, dh * 512:(dh + 1) * 512],
                                op0=ALU.mult, op1=ALU.add), r=[f"bk{ob}", "gm_all", f"yac{tt}{dh}"], w=[f"yac{tt}{dh}"])
                for tt in range(4):
                    ti = half * 4 + tt
                    pa = tt % 2
                    yk = [f"yac{tt}0", f"yac{tt}1"]
                    S.dma("sp", f"x1l{pa}", lambda e, ti=ti, pa=pa: e.dma_start(out=x1t[:, pa, :], in_=x1d[ti, :, :]),
                          w=[f"x1t{pa}"])
                    sq = st3[:, pa:pa + 1]
                    S.op("pool", lambda e, sq=sq: e.memset(sq, 0.0), w=[f"sq{pa}"])
                    S.op("act", lambda e, tt=tt, sq=sq: e.activation(out=jk[:], in_=yac[:, tt, :], func=AF.Square, accum_out=sq),
                         r=yk + ["yac"], w=["jk", f"sq{pa}"])
                    S.op("act", lambda e, sq=sq: e.activation(out=sq, in_=sq, func=AF.Ln, scale=1.0 / D, bias=epsc3),
                         r=[f"sq{pa}"], w=[f"sq{pa}"])
                    S.op("act", lambda e, sq=sq: e.activation(out=sq, in_=sq, func=AF.Exp, scale=-0.5),
                         r=[f"sq{pa}"], w=[f"sq{pa}"])
                    S.op("dve", lambda e, tt=tt, sq=sq: e.scalar_tensor_tensor(
                        out=yac[:, tt, :], in0=yac[:, tt, :], scalar=sq, in1=mod[:, GG2], op0=ALU.mult, op1=ALU.mult),
                        r=yk + [f"sq{pa}"], w=yk)
                    S.op("dve", lambda e, tt=tt, pa=pa: e.tensor_tensor(out=x1t[:, pa, :], in0=x1t[:, pa, :], in1=yac[:, tt, :],
                                                                        op=ALU.add), r=yk + [f"x1t{pa}"], w=[f"x1t{pa}"])
                    S.dma("sp", "ost", lambda e, ti=ti, pa=pa: e.dma_start(out=out[ti, :, :], in_=x1t[:, pa, :]),
                          r=[f"x1t{pa}"], w=["outd"])
            sems = {n: gs.enter_context(nc.semaphore(f"b3_{n}")) for n in S.sem_names()}
            S.emit(sems, final_waits=[("c_ost", S.chan["ost"] * 16)])
        return nc


def _bias_table():
    slopes = np.exp2(-8.0 * np.arange(1, 9, dtype=np.float32) / 8).astype(np.float32)
    r = np.arange(128)[:, None]
    cpos = np.arange(384)[None, :] - 128
    dist = np.abs(r - cpos)
    valid = dist <= 128
    tab = np.empty((128, 8, 384), np.float32)
    for h in range(8):
        tab[:, h, :] = np.where(valid, -slopes[h] * dist.astype(np.float32), np.float32(-1e30))
    return tab


def _qperm():
    cols = []
    for m in range(4):
        cols += list(range(m * 64, (m + 1) * 64))
        cols += list(range((4 + m) * 64, (5 + m) * 64))
    return np.array(cols + list(range(512, 1792)))


def _prep(inp, stage):
    f = lambda a: np.ascontiguousarray(np.asarray(a, dtype=np.float32))
    x = f(inp["x"]); c = f(inp["c"])
    w_in = f(inp["w_in"])[0][:, _qperm()].reshape(8, 128, 1792)
    w_ada = f(inp["w_ada"])[0].reshape(8, 128, 6 * D)
    b_ada = f(inp["b_ada"])[0].reshape(1, 6 * D)
    w_out = f(inp["w_out"])[0].reshape(8, 128, D)
    w_sT = np.ascontiguousarray(f(inp["w_s"])[0].transpose(2, 0, 1))
    b_sT = np.ascontiguousarray(f(inp["b_s"])[0].T)
    w_r = f(inp["w_router"])[0].reshape(8, 128, NE)
    shared = dict(w_ada=w_ada, b_ada=b_ada, w_in=np.ascontiguousarray(w_in), w_out=w_out,
                  biast=_bias_table(), w_sT=w_sT, b_sT=b_sT, w_r=w_r, ident=np.eye(128, dtype=np.float32))
    if stage == 3:
        shared["wg"] = np.ascontiguousarray(f(inp["w_gate"])[0].reshape(NE, 8, 128, NFC, 128).transpose(0, 3, 2, 1, 4))
        shared["wu"] = np.ascontiguousarray(f(inp["w_up"])[0].reshape(NE, 8, 128, NFC, 128).transpose(0, 3, 2, 1, 4))
        shared["wd"] = f(inp["w_down"])[0].reshape(NE, NFC, 128, D)
        pp = np.arange(128)
        shared["selm"] = ((pp[:, None] // 64 == pp[None, :] // 64) & (pp[:, None] % 16 == pp[None, :] % 16)).astype(np.float32)
    if stage >= 4:
        wg_all = f(inp["w_gate"])[0].reshape(NE, 8, 128, NFC, 128)
        wu_all = f(inp["w_up"])[0].reshape(NE, 8, 128, NFC, 128)
        wd_all = f(inp["w_down"])[0].reshape(NE, NFC, 128, D)
        pp = np.arange(128)
        same = (pp[:, None] // 64 == pp[None, :] // 64) & (pp[:, None] % 16 == pp[None, :] % 16)
        shared["selm"] = same.astype(np.float32)
        qq = (pp // 16) % 4
        shared["selq"] = (same & (qq[:, None] < qq[None, :])).astype(np.float32)
        shared["iota_d"] = np.ascontiguousarray(np.broadcast_to(np.arange(CAP, dtype=np.float32), (128, CAP)))
        tk = np.zeros((128, 64, 2), np.float32)
        tk[:, :, 0] = np.arange(128)[:, None]
        tk[:, :, 1] = np.arange(64)[None, :]
        shared["tokid_d"] = tk
        shared["w2_d"] = np.array([[1.0], [128.0]], np.float32)
    maps = []
    for cid in range(8):
        b, qd = cid // 4, cid % 4
        t0 = qd * 2048
        xin = np.zeros((NT + 2, 128, D), np.float32)
        lo, hi = t0 - 128, t0 + 2048 + 128
        lo_c, hi_c = max(lo, 0), min(hi, SEQ)
        xin.reshape(-1, D)[lo_c - lo: hi_c - lo] = x[b, lo_c:hi_c]
        prow = np.zeros((128, NPROW), np.float32)
        prowA = np.zeros((128, 4 * D), np.float32)
        for name, key in (("g_pre", "norm_pre_mix"), ("g_post", "norm_post_mix"), ("g_pre2", "norm_pre_ffn"),
                          ("g_post2", "norm_post_ffn")):
            prowA[:, PROWA[name]:PROWA[name] + D] = f(inp[key])[0][None, :]
        for name, key in (("ln_g", "sgu_ln_g"), ("ln_b", "sgu_ln_b"),
                          ("g_attn", "norm_out_attn"), ("g_gmlp", "norm_out_gmlp"), ("sink", "sink")):
            v = f(inp[key])[0]
            prow[:, PROW[name]:PROW[name] + v.shape[0]] = v[None, :]
        prow[:, PROW["edge"]] = -1e30 if qd == 0 else 0.0
        prow[:, PROW["edge"] + 1] = -1e30 if qd == 3 else 0.0
        prow[:, PROW["eps"]] = EPS
        m = dict(shared)
        if stage >= 4:
            m["wg"] = np.ascontiguousarray(wg_all[2 * cid:2 * cid + 2].transpose(0, 3, 2, 1, 4))
            m["wu"] = np.ascontiguousarray(wu_all[2 * cid:2 * cid + 2].transpose(0, 3, 2, 1, 4))
            m["wd"] = np.ascontiguousarray(wd_all[2 * cid:2 * cid + 2])
            sp_ = np.zeros((128, NE), np.float32)
            pbase = np.zeros((128, 32), np.float32)
            for lp in range(4):
                el, bb = lp // 2, lp % 2
                for q_ in range(4):
                    sp_[(bb * 4 + q_) * 16 + 2 * cid + el, lp * 4 + q_] = 1.0
                pbase[:, lp * 8:(lp + 1) * 8] = bb * SEQ
            m["selpair"] = sp_
            m["pbase_d"] = pbase
            ee = np.arange(NE)
            m["cbase_d"] = np.ascontiguousarray(np.broadcast_to(
                (((ee % 2) * 2 + b) * 8 * CAP + (ee // 2) * CAP).astype(np.float32), (128, NE)))
        if stage >= 3:
            so = np.zeros((128, NE), np.float32)
            so[cid * 16 + np.arange(16), np.arange(16)] = 1.0
            m["selown"] = so
        m.update(xin=xin, cvec=np.ascontiguousarray(c[b].reshape(8, 128).T), prow=prow, prowA=prowA)
        maps.append(m)
    return maps


_CACHE = {}


def kernel(**inputs):
    stage = STAGE
    if stage not in _CACHE:
        _CACHE[stage] = build_program(stage)
    nc = _CACHE[stage]
    maps = _prep(inputs, stage)
    res = run_bass_kernel_spmd(nc, maps, core_ids=list(range(8)))
    outs = [np.asarray(r["out"], dtype=np.float32).reshape(2048, D) for r in res.results]
    full = np.stack([np.concatenate(outs[0:4], 0), np.concatenate(outs[4:8], 0)], 0)
    return full.astype(np.float32)
```
